# Optimizing a Trainium2 kernel written in Bass

```python
import math
import jax, jax.numpy as jnp
from jax import lax
import numpy as np

D_MODEL = 2048
BATCH = 4
SEQ = 4096
DEPTH = 1

MIX_WIDTH = D_MODEL
ATTN_WIDTH = MIX_WIDTH // 2
SSM_WIDTH = MIX_WIDTH - ATTN_WIDTH
N_HEADS = 8
DK = ATTN_WIDTH // N_HEADS // 2
DV = 2 * DK
Q_BLOCK = 128
N_BUCKETS = 32
MAX_DISTANCE = 128
SSM_GROUP = 16
SSM_GROUPS = SSM_WIDTH // SSM_GROUP
SSM_STATE = 64
N_EXPERT_GROUPS = 4
EXPERTS_PER_GROUP = 8
N_EXPERTS = N_EXPERT_GROUPS * EXPERTS_PER_GROUP
TOP_K = 2
D_EXPERT = D_MODEL // 4
EXPERT_BLOCK = 128
PLE_DIM = 256
PROJ_WIDTH = 3 * ATTN_WIDTH + SSM_WIDTH

kernel_name = "hymba_diffattn_s5_hiermoe_ple"


def rms_norm(x, g, eps=1e-6):
    x32 = x.astype(jnp.float32)
    y = x32 * lax.rsqrt(jnp.mean(x32 * x32, axis=-1, keepdims=True) + eps)
    return (y * g.astype(jnp.float32)).astype(x.dtype)


def t5_causal_bucket(n):
    n = jnp.maximum(n, 0)
    max_exact = N_BUCKETS // 2
    is_small = n < max_exact
    nf = jnp.maximum(n, 1).astype(jnp.float32)
    large = max_exact + (jnp.log(nf / max_exact) / math.log(MAX_DISTANCE / max_exact)
                         * (N_BUCKETS - max_exact)).astype(jnp.int32)
    large = jnp.minimum(large, N_BUCKETS - 1)
    return jnp.where(is_small, n, large)


def diff_attention(zq, zk, zv, rel_bias, lq1, lk1, lq2, lk2, subln_g, lam_init):
    bt, s_len = zq.shape[0], zq.shape[1]
    q = zq.reshape(bt, s_len, N_HEADS, 2, DK).transpose(0, 2, 3, 1, 4)
    k = zk.reshape(bt, s_len, N_HEADS, 2, DK).transpose(0, 2, 3, 1, 4)
    v = zv.reshape(bt, s_len, N_HEADS, DV).transpose(0, 2, 1, 3)
    lam = (jnp.exp(jnp.sum(lq1.astype(jnp.float32) * lk1.astype(jnp.float32)))
           - jnp.exp(jnp.sum(lq2.astype(jnp.float32) * lk2.astype(jnp.float32)))
           + lam_init)
    scale = DK ** -0.5
    kpos = jnp.arange(s_len, dtype=jnp.int32)
    table = rel_bias.astype(jnp.float32)

    def block(bi):
        start = bi * Q_BLOCK
        qb = lax.dynamic_slice_in_dim(q, start, Q_BLOCK, axis=3)
        sc = jnp.einsum('bhcqd,bhckd->bhcqk', qb, k).astype(jnp.float32) * scale
        qpos = start + jnp.arange(Q_BLOCK, dtype=jnp.int32)
        rel = qpos[:, None] - kpos[None, :]
        bias = table[t5_causal_bucket(rel)].transpose(2, 0, 1)
        sc = sc + bias[None, :, None]
        sc = jnp.where((rel >= 0)[None, None, None], sc, -1e30)
        pr = jax.nn.softmax(sc, axis=-1)
        a = pr[:, :, 0] - lam * pr[:, :, 1]
        return jnp.einsum('bhqk,bhkd->bhqd', a.astype(v.dtype), v)

    o = lax.map(block, jnp.arange(s_len // Q_BLOCK))
    o = o.transpose(1, 0, 3, 2, 4).reshape(bt, s_len, N_HEADS, DV)
    o = rms_norm(o, subln_g, eps=1e-5) * (1.0 - lam_init)
    return o.reshape(bt, s_len, N_HEADS * DV)


def _ssm_combine(e1, e2):
    ar1, ai1, br1, bi1 = e1
    ar2, ai2, br2, bi2 = e2
    ar = ar2 * ar1 - ai2 * ai1
    ai = ar2 * ai1 + ai2 * ar1
    br = ar2 * br1 - ai2 * bi1 + br2
    bi = ar2 * bi1 + ai2 * br1 + bi2
    return (ar, ai, br, bi)


def s5_ssm(u, lam_re, lam_im, log_dt, b_re, b_im, c_re, c_im, d_skip):
    bt, s_len = u.shape[0], u.shape[1]
    uf = u.reshape(bt, s_len, SSM_GROUPS, SSM_GROUP).astype(jnp.float32)
    lre = lam_re.astype(jnp.float32)
    lim = lam_im.astype(jnp.float32)
    dt = jnp.exp(log_dt.astype(jnp.float32))[:, None]
    mag = jnp.exp(lre * dt)
    ab_re = mag * jnp.cos(lim * dt)
    ab_im = mag * jnp.sin(lim * dt)
    den = lre * lre + lim * lim
    nr, ni = ab_re - 1.0, ab_im
    cr = ((nr * lre + ni * lim) / den)[..., None]
    ci = ((ni * lre - nr * lim) / den)[..., None]
    bre = b_re.astype(jnp.float32)
    bim = b_im.astype(jnp.float32)
    bb_re = cr * bre - ci * bim
    bb_im = cr * bim + ci * bre
    cre = c_re.astype(jnp.float32)
    cim = c_im.astype(jnp.float32)
    dsk = d_skip.astype(jnp.float32)

    def one(ub):
        bur = jnp.einsum('sgh,gph->sgp', ub, bb_re)
        bui = jnp.einsum('sgh,gph->sgp', ub, bb_im)
        ar = jnp.broadcast_to(ab_re, bur.shape)
        ai = jnp.broadcast_to(ab_im, bur.shape)
        _, _, xr, xi = lax.associative_scan(_ssm_combine, (ar, ai, bur, bui), axis=0)
        return (jnp.einsum('sgp,ghp->sgh', xr, cre)
                - jnp.einsum('sgp,ghp->sgh', xi, cim) + dsk * ub)

    y = lax.map(one, uf)
    return y.reshape(bt, s_len, SSM_GROUPS * SSM_GROUP).astype(u.dtype)


def hier_moe(h, w_rg, b_rg, w_re, b_re, w1, w3, w2):
    n_tok, d = h.shape
    tok = jnp.arange(n_tok, dtype=jnp.int32)
    lg = (h @ w_rg).astype(jnp.float32) + b_rg.astype(jnp.float32)
    pg = jax.nn.softmax(lg, axis=-1)
    gsel = jnp.argmax(lg, axis=-1).astype(jnp.int32)
    gate_g = pg[tok, gsel][:, None]
    le = ((h @ w_re).astype(jnp.float32) + b_re.astype(jnp.float32)).reshape(
        n_tok, N_EXPERT_GROUPS, EXPERTS_PER_GROUP)
    pe = jax.nn.softmax(le[tok, gsel], axis=-1)
    top_p, top_i = lax.top_k(pe, TOP_K)
    w = gate_g * top_p / jnp.sum(top_p, axis=-1, keepdims=True)
    eid = gsel[:, None] * EXPERTS_PER_GROUP + top_i.astype(jnp.int32)

    n_assign = n_tok * TOP_K
    flat_e = eid.reshape(-1)
    flat_w = w.reshape(-1)
    flat_t = jnp.repeat(tok, TOP_K)
    counts = jnp.bincount(flat_e, length=N_EXPERTS)
    padded = ((counts + EXPERT_BLOCK - 1) // EXPERT_BLOCK) * EXPERT_BLOCK
    pad_end = jnp.cumsum(padded)
    pad_start = pad_end - padded
    start = jnp.cumsum(counts) - counts
    order = jnp.argsort(flat_e)
    se = flat_e[order]
    dest = pad_start[se] + (jnp.arange(n_assign, dtype=jnp.int32) - start[se])
    m_rows = ((n_assign + EXPERT_BLOCK - 1) // EXPERT_BLOCK) * EXPERT_BLOCK + N_EXPERTS * EXPERT_BLOCK
    row_tok = jnp.full((m_rows,), n_tok, jnp.int32).at[dest].set(flat_t[order])
    row_w = jnp.zeros((m_rows,), jnp.float32).at[dest].set(flat_w[order])
    n_blk = m_rows // EXPERT_BLOCK
    blk_start = jnp.arange(n_blk, dtype=jnp.int32) * EXPERT_BLOCK
    blk_e = jnp.minimum(jnp.searchsorted(pad_end, blk_start, side='right'), N_EXPERTS - 1)
    h_pad = jnp.concatenate([h, jnp.zeros((1, d), h.dtype)], axis=0)
    xb = h_pad[row_tok].reshape(n_blk, EXPERT_BLOCK, d)

    def expert_block(args):
        xe, e = args
        return (jax.nn.silu(xe @ w1[e]) * (xe @ w3[e])) @ w2[e]

    yb = lax.map(expert_block, (xb, blk_e)).reshape(m_rows, d)
    y = jax.ops.segment_sum(yb * row_w[:, None].astype(yb.dtype), row_tok,
                            num_segments=n_tok + 1)
    return y[:n_tok]


def setup_inputs(seed: int = 0) -> dict:
    key = jax.random.key(seed)
    ks = jax.random.split(key, 40)
    f32 = jnp.float32
    nrm = lambda k, shape, s: jax.random.normal(k, shape, f32) * s
    gain = lambda k, shape: 1.0 + 0.01 * jax.random.normal(k, shape, f32)
    L, D, G, P, H = DEPTH, D_MODEL, SSM_GROUPS, SSM_STATE, SSM_GROUP
    n_idx = jnp.arange(P, dtype=f32)[None, None, :]
    return {
        "x": jax.random.normal(ks[0], (BATCH, SEQ, D), f32),
        "p": jax.random.normal(ks[1], (L, BATCH, SEQ, PLE_DIM), f32),
        "rel_bias": nrm(ks[2], (N_BUCKETS, N_HEADS), 0.1),
        "g_mix": gain(ks[3], (L, D)),
        "w_in": nrm(ks[4], (L, D, PROJ_WIDTH), D ** -0.5),
        "lam_q1": nrm(ks[5], (L, DK), 0.1),
        "lam_k1": nrm(ks[6], (L, DK), 0.1),
        "lam_q2": nrm(ks[7], (L, DK), 0.1),
        "lam_k2": nrm(ks[8], (L, DK), 0.1),
        "subln_g": gain(ks[9], (L, DV)),
        "ssm_lam_re": -0.5 + 0.01 * jax.random.normal(ks[10], (L, G, P), f32),
        "ssm_lam_im": math.pi * n_idx + 0.01 * jax.random.normal(ks[11], (L, G, P), f32),
        "ssm_log_dt": jax.random.uniform(ks[12], (L, G), f32, math.log(1e-3), math.log(1e-1)),
        "ssm_b_re": nrm(ks[13], (L, G, P, H), (2.0 * H) ** -0.5),
        "ssm_b_im": nrm(ks[14], (L, G, P, H), (2.0 * H) ** -0.5),
        "ssm_c_re": nrm(ks[15], (L, G, H, P), P ** -0.5),
        "ssm_c_im": nrm(ks[16], (L, G, H, P), P ** -0.5),
        "ssm_d": nrm(ks[17], (L, G, H), 1.0),
        "w_glu": nrm(ks[18], (L, SSM_WIDTH, SSM_WIDTH), SSM_WIDTH ** -0.5),
        "b_glu": nrm(ks[19], (L, SSM_WIDTH), 0.01),
        "ssm_norm_g": gain(ks[20], (L, SSM_WIDTH)),
        "w_o": nrm(ks[21], (L, MIX_WIDTH, D), MIX_WIDTH ** -0.5),
        "g_ffn": gain(ks[22], (L, D)),
        "w_router_g": nrm(ks[23], (L, D, N_EXPERT_GROUPS), D ** -0.5),
        "b_router_g": nrm(ks[24], (L, N_EXPERT_GROUPS), 0.01),
        "w_router_e": nrm(ks[25], (L, D, N_EXPERTS), D ** -0.5),
        "b_router_e": nrm(ks[26], (L, N_EXPERTS), 0.01),
        "w1": nrm(ks[27], (L, N_EXPERTS, D, D_EXPERT), D ** -0.5),
        "w3": nrm(ks[28], (L, N_EXPERTS, D, D_EXPERT), D ** -0.5),
        "w2": nrm(ks[29], (L, N_EXPERTS, D_EXPERT, D), D_EXPERT ** -0.5),
        "g_ple": gain(ks[30], (L, D)),
        "w_ple_gate": nrm(ks[31], (L, D, D), D ** -0.5),
        "w_ple_proj": nrm(ks[32], (L, PLE_DIM, D), PLE_DIM ** -0.5),
        "g_final": gain(ks[33], (D,)),
    }


def reference(x, p, rel_bias, g_mix, w_in, lam_q1, lam_k1, lam_q2, lam_k2, subln_g,
              ssm_lam_re, ssm_lam_im, ssm_log_dt, ssm_b_re, ssm_b_im, ssm_c_re, ssm_c_im,
              ssm_d, w_glu, b_glu, ssm_norm_g, w_o, g_ffn, w_router_g, b_router_g,
              w_router_e, b_router_e, w1, w3, w2, g_ple, w_ple_gate, w_ple_proj, g_final):
    bt, s_len, d = x.shape
    for i in range(DEPTH):
        lam_init = 0.8 - 0.6 * math.exp(-0.3 * i)
        h = rms_norm(x, g_mix[i])
        z = h @ w_in[i]
        zq = z[..., :ATTN_WIDTH]
        zk = z[..., ATTN_WIDTH:2 * ATTN_WIDTH]
        zv = z[..., 2 * ATTN_WIDTH:3 * ATTN_WIDTH]
        zu = z[..., 3 * ATTN_WIDTH:]
        a = diff_attention(zq, zk, zv, rel_bias, lam_q1[i], lam_k1[i], lam_q2[i],
                           lam_k2[i], subln_g[i], lam_init)
        s = s5_ssm(zu, ssm_lam_re[i], ssm_lam_im[i], ssm_log_dt[i], ssm_b_re[i],
                   ssm_b_im[i], ssm_c_re[i], ssm_c_im[i], ssm_d[i])
        s = jax.nn.gelu(s)
        s = s * jax.nn.sigmoid(s @ w_glu[i] + b_glu[i])
        s = rms_norm(s, ssm_norm_g[i])
        x = x + jnp.concatenate([a, s], axis=-1) @ w_o[i]
        h = rms_norm(x, g_ffn[i]).reshape(bt * s_len, d)
        x = x + hier_moe(h, w_router_g[i], b_router_g[i], w_router_e[i], b_router_e[i],
                         w1[i], w3[i], w2[i]).reshape(bt, s_len, d)
        gate = jax.nn.sigmoid(rms_norm(x, g_ple[i]) @ w_ple_gate[i])
        x = x + gate * (p[i] @ w_ple_proj[i])
    return rms_norm(x, g_final)
```

```python
import contextlib
import math
import numpy as np
import concourse.bass as bass
import concourse.mybir as mybir
from concourse.bass_utils import run_bass_kernel_spmd

F32 = mybir.dt.float32
BF16 = mybir.dt.bfloat16
I32 = mybir.dt.int32
U32 = mybir.dt.uint32
AF = mybir.ActivationFunctionType
ALU = mybir.AluOpType
AX = mybir.AxisListType


class Buf:
    __slots__ = ("name", "t", "writers", "readers", "dsem")

    def __init__(self, name, t=None):
        self.name = name
        self.t = t
        self.writers = {}
        self.readers = {}
        self.dsem = None

    def __getitem__(self, idx):
        return self.t[idx]


class K:
    ENGS = ("pe", "act", "dve", "pool", "sp")

    def __init__(self, nc, stack):
        self.nc = nc
        self.stack = stack
        self.main_stack = stack
        self.free_dsems = []
        self.phase_bufs = None
        self.ops = {e: [] for e in self.ENGS}
        self.sems = {}
        self.count = {}
        self.seen = {e: {} for e in self.ENGS}
        self.pending = {e: False for e in self.ENGS}
        for e in self.ENGS:
            self._mksem("E_" + e)

    def _mksem(self, key):
        if key.startswith("D") and self.free_dsems:
            return self.free_dsems.pop()
        s = self.main_stack.enter_context(self.nc.semaphore("s_" + key))
        self.sems[key] = s
        self.count[key] = 0
        return key

    def sb(self, name, shape, dtype):
        self._n = getattr(self, "_n", 0) + 1
        name = "%s_%d" % (name, self._n)
        t = self.stack.enter_context(self.nc.sbuf_tensor(name, list(shape), dtype))
        b = Buf(name, t)
        if self.phase_bufs is not None:
            self.phase_bufs.append(b)
        return b

    def ps(self, name, shape, dtype=F32):
        self._n = getattr(self, "_n", 0) + 1
        name = "%s_%d" % (name, self._n)
        t = self.stack.enter_context(self.nc.psum_tensor(name, list(shape), dtype))
        b = Buf(name, t)
        if self.phase_bufs is not None:
            self.phase_bufs.append(b)
        return b

    def dram(self, name, shape, dtype, kind="Internal"):
        t = self.nc.dram_tensor(name, list(shape), dtype, kind=kind)
        return Buf(name, t.ap())

    def _waits(self, eng, reads, writes):
        need = {}
        for b in reads:
            for k, v in b.writers.items():
                if need.get(k, 0) < v:
                    need[k] = v
        for b in writes:
            for k, v in b.writers.items():
                if need.get(k, 0) < v:
                    need[k] = v
            for k, v in b.readers.items():
                if need.get(k, 0) < v:
                    need[k] = v
        out = []
        seen = self.seen[eng]
        own = "E_" + eng
        for k, v in need.items():
            if eng == "pe" and k == own:
                continue
            if seen.get(k, 0) < v:
                seen[k] = v
                out.append((k, v))
        return out

    def op(self, eng, fn, reads=(), writes=(), inc=True):
        waits = self._waits(eng, reads, writes)
        key = "E_" + eng
        val = self.count[key] + 1
        if inc:
            self.count[key] = val
            self.pending[eng] = False
        else:
            self.pending[eng] = True
        self.ops[eng].append((waits, fn, key if inc else None, 1))
        for b in writes:
            b.writers = {key: val}
            b.readers = {}
        for b in reads:
            if b.readers.get(key, 0) < val:
                b.readers[key] = val

    def dma(self, q, parts, sbuf, reads=(), writes=()):
        waits = self._waits(q, reads, writes)
        if sbuf.dsem is None:
            sbuf.dsem = self._mksem("D%d_%s" % (len(self.sems), sbuf.name))
        key = sbuf.dsem
        first = True
        for p in parts:
            o, i = p[0], p[1]
            kw = p[2] if len(p) > 2 else {}
            self.count[key] += 16
            fn = (lambda e, o=o, i=i, kw=kw: e.dma_start(out=o, in_=i, **kw))
            self.ops[q].append((waits if first else [], fn, key, 16))
            first = False
        val = self.count[key]
        for b in writes:
            b.writers = {key: val}
            b.readers = {}
        for b in reads:
            if b.readers.get(key, 0) < val:
                b.readers[key] = val

    def dma_fn(self, q, fns, sbuf, reads=(), writes=()):
        waits = self._waits(q, reads, writes)
        if sbuf.dsem is None:
            sbuf.dsem = self._mksem("D%d_%s" % (len(self.sems), sbuf.name))
        key = sbuf.dsem
        first = True
        for fn in fns:
            self.count[key] += 16
            self.ops[q].append((waits if first else [], fn, key, 16))
            first = False
        val = self.count[key]
        for b in writes:
            b.writers = {key: val}
            b.readers = {}
        for b in reads:
            if b.readers.get(key, 0) < val:
                b.readers[key] = val

    def barrier(self):
        for e in self.ENGS:
            assert not self.pending[e], e
        for e in self.ENGS:
            waits = []
            seen = self.seen[e]
            for key, cnt in self.count.items():
                if cnt > 0 and seen.get(key, 0) < cnt and not (e == "pe" and key == "E_pe"):
                    seen[key] = cnt
                    waits.append((key, cnt))
            if waits:
                self.ops[e].append((waits, None, None, 0))

    @contextlib.contextmanager
    def phase(self):
        outer = self.stack
        outer_bufs = self.phase_bufs
        with contextlib.ExitStack() as ph:
            self.stack = ph
            self.phase_bufs = []
            yield
            self.barrier()
            for b in self.phase_bufs:
                if b.dsem is not None:
                    self.free_dsems.append(b.dsem)
                    b.dsem = None
            self.phase_bufs = outer_bufs
            self.stack = outer
            self.flush()

    def flush(self):
        for e in self.ENGS:
            assert not self.pending[e], "engine %s ends with non-incrementing op" % e

        def run(engname):
            lst = self.ops[engname]

            def body(eng):
                for waits, fn, key, n in lst:
                    for k, v in waits:
                        eng.wait_ge(self.sems[k], v)
                    if fn is None:
                        continue
                    ins = fn(eng)
                    if key is not None:
                        ins.then_inc(self.sems[key], n)
            return body

        with self.nc.Block() as block:
            block.sync(run("sp"))
            block.scalar(run("act"))
            block.vector(run("dve"))
            block.gpsimd(run("pool"))
            block.tensor(run("pe"))
        self.ops = {e: [] for e in self.ENGS}

    def finish(self, final_waits):
        for b in final_waits:
            w = self._waits("sp", [b], [b])
            self.ops["sp"].append((w, None, None, 0))
        self.barrier()
        self.flush()


def cp(k, eng, out, in_, reads, writes, inc=True):
    if eng == "act":
        k.op("act", lambda e: e.copy(out=out, in_=in_), reads, writes, inc)
    else:
        k.op(eng, lambda e: e.tensor_copy(out=out, in_=in_), reads, writes, inc)


D = 2048
S = 4096
NT = 32
NOWN = 2048
AW = 1024
NH = 8
DK = 64
DV = 128
NG = 64
NE = 32
DE = 512
CAP = 256
LAM_INIT = 0.8 - 0.6 * math.exp(-0.3 * 0)
EPS = 1e-6


def _t5_bucket(n):
    n = np.maximum(n, 0)
    nf = np.maximum(n, 1).astype(np.float32)
    large = 16 + (np.log(nf / np.float32(16)) / np.float32(math.log(128 / 16)) * np.float32(16)).astype(np.int32)
    large = np.minimum(large, 31)
    return np.where(n < 16, n, large)


def _lag_onehot(hf):
    oh = np.zeros((33, 768), np.float32)
    for m in range(3):
        delta = hf + 1 - m
        for n1 in range(256):
            lag = delta * 128 + n1 - 128
            c = m * 256 + n1
            if lag < 0:
                oh[32, c] = 1.0
            else:
                oh[int(_t5_bucket(np.array([lag]))[0]), c] += 1.0
                oh[31, c] -= 1.0
    return oh


def own_token_perm(hf):
    n = np.arange(NOWN)
    glob = 128 * (2 * (n // 128) + hf) + (n % 128)
    cb, l = n // 1024, n % 1024
    pos = cb * 1024 + (l % 8) * 128 + (l // 8)
    perm = np.zeros(NOWN, np.int64)
    perm[pos] = glob
    return perm


def build_nc(upto=99, dbg=()):
    nc = bass.Bass("TRN2", target_bir_lowering=False)
    I = {}

    def inp(name, shape, dt=F32):
        I[name] = nc.dram_tensor(name, list(shape), dt, kind="ExternalInput").ap()
        return I[name]

    xg = inp("xg", [S, D])
    xo = inp("xo", [NOWN, D])
    po = inp("po", [NOWN, 256])
    cc = inp("cc", [128, 4])
    ohlag = inp("ohlag", [33, 768])
    rel_bias = inp("rel_bias", [32, 8])
    g_mix = inp("g_mix", [D])
    w_in = inp("w_in", [D, 4096])
    lamv = inp("lamv", [4, 64])
    subln_g = inp("subln_g", [128])
    ssm_lam_re = inp("ssm_lam_re", [64, 64])
    ssm_lam_im = inp("ssm_lam_im", [64, 64])
    ssm_log_dt = inp("ssm_log_dt", [64])
    ssm_b_re = inp("ssm_b_re", [64, 64, 16])
    ssm_b_im = inp("ssm_b_im", [64, 64, 16])
    ssm_c_re = inp("ssm_c_re", [64, 16, 64])
    ssm_c_im = inp("ssm_c_im", [64, 16, 64])
    ssm_d = inp("ssm_d", [64, 16])
    w_glu = inp("w_glu", [1024, 1024])
    b_glu = inp("b_glu", [1024])
    ssm_norm_g = inp("ssm_norm_g", [1024])
    w_o = inp("w_o", [D, D])
    g_ffn = inp("g_ffn", [D])
    wr = inp("wr", [D, 36])
    br = inp("br", [36])
    w1 = inp("w1", [NE, D, DE])
    w3 = inp("w3", [NE, D, DE])
    w2 = inp("w2", [NE, DE, D])
    g_ple = inp("g_ple", [D])
    w_ple_gate = inp("w_ple_gate", [D, D])
    w_ple_proj = inp("w_ple_proj", [256, D])
    g_final = inp("g_final", [D])
    O = {}

    def outp(name, shape, dt=F32):
        O[name] = nc.dram_tensor(name, list(shape), dt, kind="ExternalOutput").ap()
        return O[name]

    with contextlib.ExitStack() as st:
        k = K(nc, st)
        hT_d = k.dram("hT_d", [NT, 128, 16, 128], BF16)
        QT_d = k.dram("QT_d", [NH, 128, S], BF16)
        KT_d = k.dram("KT_d", [NH, 128, S], BF16)
        V_d = k.dram("V_d", [S, AW], BF16)
        U_d = k.dram("U_d", [8, 16, NG, 512], BF16)
        E_d = k.dram("E_d", [NH, 3, 129 * 256], F32)
        catT_d = k.dram("catT_d", [16, 128, NOWN], BF16)
        X1_d = k.dram("X1_d", [NOWN, D], F32)
        NROW = NE * CAP
        H_d = k.dram("H_d", [NROW, D], BF16)
        Y_dA = k.dram("Y_dA", [NROW, D // 2], F32)
        Y_dB = k.dram("Y_dB", [NROW, D // 2], F32)
        dest_i = k.sb("dest_i", [128, 16, 2], I32)
        wts = k.sb("wts", [128, 16, 2], F32)
        idxin = k.sb("idxin", [128, NE, 4], I32)
        idxout = k.sb("idxout", [128, NE, 4], I32)
        H2_d = k.dram("H2_d", [NOWN, D], BF16)
        ident = k.sb("ident", [128, 128], BF16)
        identf = k.sb("identf", [128, 128], F32)
        ccs = k.sb("ccs", [128, 4], F32)
        lam_t = k.sb("lam_t", [128, 1], F32)
        E_sb = k.sb("E_sb", [128, NH, 3, 128], F32)
        final = []
        _bc = {}

        def bcreg(e):
            if "r" not in _bc:
                _bc["r"] = e.to_reg(NE * CAP - 1)
            return _bc["r"]

        with k.phase():
            k.op("pool", lambda e: e.memset(identf[:], 0.0), writes=[identf])
            k.op("pool", lambda e: e.affine_select(out=identf[:], in_=identf[:], pattern=[[-1, 128]],
                                                   compare_op=ALU.not_equal, fill=1.0, base=0,
                                                   channel_multiplier=1), reads=[identf], writes=[identf])
            cp(k, "dve", ident[:], identf[:], [identf], [ident])
            k.dma("sp", [(ccs[:], cc)], ccs, writes=[ccs])
            lv = k.sb("lv", [128, 4, 64], F32)
            k.dma("sp", [(lv[:], lamv.rearrange("a b -> (a b)").partition_broadcast(128).rearrange("p (a b) -> p a b", a=4))], lv, writes=[lv])
            lj = k.sb("lj", [128, 64], F32)
            l2 = k.sb("l2", [128, 2], F32)
            for i in range(2):
                k.op("dve", lambda e, i=i: e.tensor_tensor(out=lj[:], in0=lv[:, 2 * i, :], in1=lv[:, 2 * i + 1, :], op=ALU.mult), reads=[lv], writes=[lj])
                k.op("dve", lambda e, i=i: e.tensor_reduce(out=l2[:, i:i + 1], in_=lj[:], axis=AX.X, op=ALU.add), reads=[lj], writes=[l2])
            k.op("act", lambda e: e.activation(out=l2[:], in_=l2[:], func=AF.Exp), reads=[l2], writes=[l2])
            k.op("dve", lambda e: e.tensor_tensor(out=lam_t[:], in0=l2[:, 0:1], in1=l2[:, 1:2], op=ALU.subtract), reads=[l2], writes=[lam_t])
            k.op("dve", lambda e: e.tensor_scalar(out=lam_t[:], in0=lam_t[:], scalar1=LAM_INIT, scalar2=None, op0=ALU.add), reads=[lam_t], writes=[lam_t])
            tb = k.sb("tb", [64, 8], F32)
            k.op("pool", lambda e: e.memset(tb[:], -30000.0), writes=[tb])
            k.dma("sp", [(tb[0:32, :], rel_bias)], tb, reads=[tb], writes=[tb])
            ohs = k.sb("ohs", [64, 768], F32)
            k.op("pool", lambda e: e.memset(ohs[:], 0.0), writes=[ohs])
            k.dma("sp", [(ohs[0:33, :], ohlag)], ohs, reads=[ohs], writes=[ohs])
            pe_ = k.ps("pe_", [8, 768], F32)
            k.op("pe", lambda e: e.matmul(pe_[:, 0:384], lhsT=tb[:], rhs=ohs[:, 0:384], start=True, stop=True), reads=[tb, ohs], writes=[pe_], inc=False)
            k.op("pe", lambda e: e.matmul(pe_[:, 384:768], lhsT=tb[:], rhs=ohs[:, 384:768], start=True, stop=True), reads=[tb, ohs], writes=[pe_])
            es = k.sb("es", [8, 768], F32)
            k.op("act", lambda e: e.activation(out=es[:], in_=pe_[:], func=AF.Exp), reads=[pe_], writes=[es])
            parts = []
            for m in range(3):
                parts.append((E_d[:, m, :].rearrange("h (r n) -> h r n", n=256),
                              es[:, m * 256:(m + 1) * 256].unsqueeze(1).to_broadcast([8, 129, 256])))
            k.dma("sp", parts, es, reads=[es], writes=[E_d])
            parts = []
            for h in range(NH):
                for m in range(3):
                    src = E_d[h, m]
                    parts.append((E_sb[:, h, m, :], bass.AP(src.tensor, src.offset + 128, [[255, 128], [1, 128]])))
            k.dma("sp", parts, E_sb, reads=[E_d], writes=[E_sb])
            if "E" in dbg:
                k.dma("sp", [(outp("dbg_E", [128, NH * 3 * 128]), E_sb[:].rearrange("p a b c -> p (a b c)"))], E_sb, reads=[E_sb])
                k.dma("sp", [(outp("dbg_lam", [128, 1]), lam_t[:])], lam_t, reads=[lam_t])
                final += [E_sb, lam_t]

        def sin_red(outb, out_ap, angb, ang_ap, shape, tmps):
            tq, ti, tr, tm = tmps
            sl = tuple(slice(0, n) for n in shape)
            k.op("dve", lambda e: e.tensor_scalar(out=tq[sl], in0=ang_ap, scalar1=1.0 / 6.283185307179586, scalar2=None, op0=ALU.mult), reads=[angb], writes=[tq])
            k.op("dve", lambda e: e.tensor_copy(out=ti[sl], in_=tq[sl]), reads=[tq], writes=[ti])
            k.op("dve", lambda e: e.tensor_copy(out=tq[sl], in_=ti[sl]), reads=[ti], writes=[tq])
            k.op("dve", lambda e: e.scalar_tensor_tensor(out=tr[sl], in0=tq[sl], scalar=-6.283185307179586, in1=ang_ap, op0=ALU.mult, op1=ALU.add), reads=[tq, angb], writes=[tr])
            k.op("dve", lambda e: e.tensor_scalar(out=tm[sl], in0=tr[sl], scalar1=math.pi, scalar2=-6.283185307179586, op0=ALU.is_gt, op1=ALU.mult), reads=[tr], writes=[tm])
            k.op("dve", lambda e: e.tensor_tensor(out=tr[sl], in0=tr[sl], in1=tm[sl], op=ALU.add), reads=[tr, tm], writes=[tr])
            k.op("dve", lambda e: e.tensor_scalar(out=tm[sl], in0=tr[sl], scalar1=-math.pi, scalar2=6.283185307179586, op0=ALU.is_lt, op1=ALU.mult), reads=[tr], writes=[tm])
            k.op("dve", lambda e: e.tensor_tensor(out=tr[sl], in0=tr[sl], in1=tm[sl], op=ALU.add), reads=[tr, tm], writes=[tr])
            k.op("dve", lambda e: e.tensor_scalar(out=tr[sl], in0=tr[sl], scalar1=-3.1415925, scalar2=3.1415925, op0=ALU.max, op1=ALU.min), reads=[tr], writes=[tr])
            k.op("act", lambda e: e.activation(out=out_ap, in_=tr[sl], func=AF.Sin), reads=[tr], writes=[outb])

        if upto >= 1:
          with k.phase():
            gm = k.sb("gm", [128, D], F32)
            k.dma("act", [(gm[:], g_mix.partition_broadcast(128))], gm, writes=[gm])
            xt = [k.sb("xt%d" % i, [128, D], F32) for i in range(2)]
            hb = [k.sb("hb%d" % i, [128, D], BF16) for i in range(2)]
            junk = k.sb("junk", [128, D], BF16)
            ssq = [k.sb("ssq%d" % i, [128, 1], F32) for i in range(2)]
            hTs = [k.sb("hTs%d" % i, [128, 16, 128], BF16) for i in range(2)]
            pT = [k.ps("pT%d" % i, [128, 8, 128], BF16) for i in range(2)]
            for i in range(NT):
                s = i % 2
                k.dma("sp", [(xt[s][:], xg[i * 128:(i + 1) * 128, :])], xt[s], writes=[xt[s]])
                k.op("act", lambda e, s=s: e.activation(out=junk[:], in_=xt[s][:], func=AF.Square, accum_out=ssq[s][:]),
                     reads=[xt[s]], writes=[junk, ssq[s]])
                k.op("dve", lambda e, s=s: e.tensor_scalar(out=ssq[s][:], in0=ssq[s][:], scalar1=1.0 / D, scalar2=EPS, op0=ALU.mult, op1=ALU.add),
                     reads=[ssq[s]], writes=[ssq[s]])
                k.op("act", lambda e, s=s: e.activation(out=ssq[s][:], in_=ssq[s][:], func=AF.Sqrt), reads=[ssq[s]], writes=[ssq[s]])
                k.op("dve", lambda e, s=s: e.reciprocal(out=ssq[s][:], in_=ssq[s][:]), reads=[ssq[s]], writes=[ssq[s]])
                k.op("dve", lambda e, s=s: e.scalar_tensor_tensor(out=hb[s][:], in0=xt[s][:], scalar=ssq[s][:], in1=gm[:], op0=ALU.mult, op1=ALU.mult),
                     reads=[xt[s], ssq[s], gm], writes=[hb[s]])
                for half in range(2):
                    for c in range(8):
                        kc = half * 8 + c
                        k.op("pe", lambda e, s=s, half=half, c=c, kc=kc: e.transpose(out=pT[half][:, c, :], in_=hb[s][:, kc * 128:(kc + 1) * 128], identity=ident[:]),
                             reads=[hb[s], ident], writes=[pT[half]], inc=(c == 7))
                    cp(k, "act" if half == 0 else "dve", hTs[s][:, half * 8:(half + 1) * 8, :], pT[half][:], [pT[half]], [hTs[s]])
                k.dma("act", [(hT_d[i], hTs[s][:])], hTs[s], reads=[hTs[s]], writes=[hT_d])

        if upto >= 2:
          with k.phase():
            wb = [k.sb("wb%d" % i, [128, 16, 512], BF16) for i in range(2)]
            hs = [k.sb("hs%d" % i, [128, 4, 16, 128], BF16) for i in range(2)]
            pz = [k.ps("pz%d" % i, [128, 512], F32) for i in range(4)]
            stg = [k.sb("stg%d" % i, [128, 4, 512], BF16) for i in range(2)]
            usb = k.sb("usb", [128, 4, 8, 512], BF16)
            w_v = w_in.rearrange("(kc p) n -> p kc n", p=128)
            nz = 0
            nh = 0
            for cb in range(8):
                ws = wb[cb % 2]
                k.dma("pool", [(ws[:, 0:8, :], w_v[:, 0:8, cb * 512:(cb + 1) * 512]), (ws[:, 8:16, :], w_v[:, 8:16, cb * 512:(cb + 1) * 512])], ws, writes=[ws])
                kind = "QKVU"[cb // 2]
                for stile in range(8):
                    hss = hs[nh % 2]
                    nh += 1
                    k.dma("sp", [(hss[:], hT_d[stile * 4:(stile + 1) * 4].rearrange("t p c n -> p t c n"))], hss, reads=[hT_d], writes=[hss])
                    sg = stg[stile % 2]
                    for m in range(4):
                        pp = pz[nz % 4]
                        nz += 1
                        ev = "act" if nz % 2 == 0 else "dve"
                        if kind == "V":
                            for kc in range(16):
                                k.op("pe", lambda e, pp=pp, hss=hss, ws=ws, m=m, kc=kc: e.matmul(pp[:], lhsT=hss[:, m, kc, :], rhs=ws[:, kc, :], start=(kc == 0), stop=(kc == 15)),
                                     reads=[hss, ws], writes=[pp], inc=(kc == 15))
                            cp(k, ev, sg[:, m, :], pp[:], [pp], [sg])
                        else:
                            for kc in range(16):
                                k.op("pe", lambda e, pp=pp, hss=hss, ws=ws, m=m, kc=kc: e.matmul(pp[:].rearrange("p (t n) -> p t n", t=4), lhsT=ws[:, kc, m * 128:(m + 1) * 128], rhs=hss[:, :, kc, :], start=(kc == 0), stop=(kc == 15)),
                                     reads=[hss, ws], writes=[pp], inc=(kc == 15))
                            if kind == "Q":
                                if ev == "act":
                                    k.op("act", lambda e, sg=sg, pp=pp, m=m: e.activation(out=sg[:, m, :], in_=pp[:], func=AF.Copy, scale=DK ** -0.5), reads=[pp], writes=[sg])
                                else:
                                    k.op("dve", lambda e, sg=sg, pp=pp, m=m: e.tensor_scalar(out=sg[:, m, :], in0=pp[:], scalar1=DK ** -0.5, scalar2=None, op0=ALU.mult), reads=[pp], writes=[sg])
                            elif kind == "K":
                                cp(k, ev, sg[:, m, :], pp[:], [pp], [sg])
                            else:
                                cp(k, ev, usb[:, m, :, stile * 64:(stile + 1) * 64], pp[:].rearrange("p (c s) -> p s c", s=8), [pp], [usb])
                    t0 = stile * 512
                    if kind == "Q" or kind == "K":
                        dst = QT_d if kind == "Q" else KT_d
                        h0 = (cb % 2) * 4
                        k.dma("act", [(dst[h0:h0 + 4, :, t0:t0 + 512].rearrange("h p n -> p h n"), sg[:])], sg, reads=[sg], writes=[dst])
                    elif kind == "V":
                        c0 = (cb % 2) * 512
                        k.dma("act", [(V_d[t0:t0 + 512, c0:c0 + 512].rearrange("(t p) n -> p t n", p=128), sg[:])], sg, reads=[sg], writes=[V_d])
                if kind == "U":
                    g0 = (cb % 2) * 32
                    parts = []
                    for m in range(4):
                        for gl in range(8):
                            parts.append((U_d[:, :, g0 + 8 * m + gl, :].rearrange("s h c -> h s c"), usb[gl * 16:(gl + 1) * 16, m, :, :]))
                    k.dma("act", parts, usb, reads=[usb], writes=[U_d])
        if upto >= 3:
          with k.phase():
            gsub = k.sb("gsub", [128, 128], F32)
            k.dma("sp", [(gsub[:], subln_g.partition_broadcast(128))], gsub, writes=[gsub])
            k.op("dve", lambda e: e.tensor_scalar(out=gsub[:], in0=gsub[:], scalar1=1.0 - LAM_INIT, scalar2=None, op0=ALU.mult), reads=[gsub], writes=[gsub])
            KTs = [k.sb("KTs%d" % i, [128, S], BF16) for i in range(2)]
            QTa = [k.sb("QTa%d" % i, [128, NT, 128], BF16) for i in range(2)]
            QTo = [k.sb("QTo%d" % i, [128, 16, 128], BF16) for i in range(2)]
            Vh = [k.sb("Vh%d" % i, [128, NT, 132], BF16) for i in range(2)]
            aTh = [k.sb("aTh%d" % i, [128, NOWN], BF16) for i in range(2)]
            for i in range(2):
                k.op("pool", lambda e, i=i: e.memset(Vh[i][:], 1.0), writes=[Vh[i]])
            S1 = [k.ps("S1_%d" % i, [128, 4, 128], F32) for i in range(2)]
            S2 = [k.ps("S2_%d" % i, [128, 4, 128], F32) for i in range(2)]
            O1 = k.ps("O1", [128, 512], F32)
            O2 = k.ps("O2", [128, 512], F32)
            pA = k.ps("pA", [128, 128], BF16)
            P1 = [k.sb("P1_%d" % i, [128, 4, 128], BF16) for i in range(2)]
            P2 = [k.sb("P2_%d" % i, [128, 4, 128], BF16) for i in range(2)]
            Pf1 = k.sb("Pf1", [128, 3, 128], F32)
            Pf2 = k.sb("Pf2", [128, 3, 128], F32)
            rr = k.sb("rr", [128, 2], F32)
            tmpo = k.sb("tmpo", [128, 128], F32)
            oo = k.sb("oo", [128, 128], F32)
            jk = k.sb("jk", [128, 128], F32)
            ms = k.sb("ms", [128, 1], F32)
            ab = k.sb("ab", [128, 128], BF16)
            ng = 0
            for h in range(NH):
                hs_ = h % 2
                k.dma("sp", [(KTs[hs_][:], KT_d[h])], KTs[hs_], reads=[KT_d], writes=[KTs[hs_]])
                k.dma("sp", [(QTa[hs_][:].rearrange("p t n -> p (t n)"), QT_d[h])], QTa[hs_], reads=[QT_d], writes=[QTa[hs_]])
                k.dma("act", [(Vh[hs_][:, :, 0:128], V_d[:, h * 128:(h + 1) * 128].rearrange("(t p) n -> p t n", p=128))], Vh[hs_], reads=[V_d], writes=[Vh[hs_]])
                qv = QTa[hs_][:].rearrange("p (j r) n -> p j r n", r=2)
                k.op("pool", lambda e, hs_=hs_, qv=qv: e.tensor_scalar(out=QTo[hs_][:], in0=qv[:, :, 0, :], scalar1=ccs[:, 0:1], scalar2=None, op0=ALU.mult),
                     reads=[QTa[hs_], ccs], writes=[QTo[hs_]])
                k.op("dve", lambda e, hs_=hs_, qv=qv: e.scalar_tensor_tensor(out=QTo[hs_][:], in0=qv[:, :, 1, :], scalar=ccs[:, 1:2], in1=QTo[hs_][:], op0=ALU.mult, op1=ALU.add),
                     reads=[QTa[hs_], ccs, QTo[hs_]], writes=[QTo[hs_]])
                for j in range(16):
                    nplain = max(0, 2 * j - 1)
                    groups = [list(range(a, min(a + 4, nplain))) for a in range(0, nplain, 4)]
                    spec = [kb for kb in (2 * j - 1, 2 * j, 2 * j + 1) if kb >= 0]
                    groups.append(spec)
                    nkb = 2 * j + 2
                    for gi, grp in enumerate(groups):
                        is_spec = (gi == len(groups) - 1)
                        s_ = ng % 2
                        ng += 1
                        for mp, (SS, base) in enumerate(((S1[s_], 0), (S2[s_], 64))):
                            for i, kb in enumerate(grp):
                                k.op("pe", lambda e, SS=SS, base=base, i=i, kb=kb, hs_=hs_, j=j: e.matmul(SS[:, i, :], lhsT=KTs[hs_][base:base + 64, kb * 128:(kb + 1) * 128], rhs=QTo[hs_][base:base + 64, j, :], start=True, stop=True),
                                     reads=[KTs[hs_], QTo[hs_]], writes=[SS], inc=(i == len(grp) - 1))
                        n = len(grp)
                        for mp, (SS, PP, Pf) in enumerate(((S1[s_], P1[s_], Pf1), (S2[s_], P2[s_], Pf2))):
                            if not is_spec:
                                k.op("act", lambda e, SS=SS, PP=PP, n=n: e.activation(out=PP[:, 0:n, :], in_=SS[:, 0:n, :], func=AF.Exp), reads=[SS], writes=[PP])
                            else:
                                k.op("act", lambda e, SS=SS, Pf=Pf, n=n: e.activation(out=Pf[:, 0:n, :], in_=SS[:, 0:n, :], func=AF.Exp), reads=[SS], writes=[Pf])
                                m0 = 3 - n
                                k.op("dve", lambda e, PP=PP, Pf=Pf, n=n, m0=m0, h=h: e.tensor_tensor(out=PP[:, 0:n, :], in0=Pf[:, 0:n, :], in1=E_sb[:, h, m0:3, :], op=ALU.mult),
                                     reads=[Pf, E_sb], writes=[PP])
                        for mp, (PP, OO) in enumerate(((P1[s_], O1), (P2[s_], O2))):
                            for i, kb in enumerate(grp):
                                k.op("pe", lambda e, PP=PP, OO=OO, i=i, kb=kb, hs_=hs_, nkb=nkb: e.matmul(OO[:, 0:129], lhsT=PP[:, i, :], rhs=Vh[hs_][:, kb, 0:129], start=(kb == 0), stop=(kb == nkb - 1)),
                                     reads=[PP, Vh[hs_]], writes=[OO], inc=(i == len(grp) - 1))
                    k.op("dve", lambda e: e.reciprocal(out=rr[:, 0:1], in_=O1[:, 128:129]), reads=[O1], writes=[rr])
                    k.op("dve", lambda e: e.reciprocal(out=rr[:, 1:2], in_=O2[:, 128:129]), reads=[O2, rr], writes=[rr])
                    k.op("dve", lambda e: e.tensor_tensor(out=rr[:, 1:2], in0=rr[:, 1:2], in1=lam_t[:], op=ALU.mult), reads=[rr, lam_t], writes=[rr])
                    k.op("dve", lambda e: e.tensor_scalar(out=tmpo[:], in0=O2[:, 0:128], scalar1=rr[:, 1:2], scalar2=None, op0=ALU.mult), reads=[O2, rr], writes=[tmpo])
                    k.op("dve", lambda e: e.scalar_tensor_tensor(out=oo[:], in0=O1[:, 0:128], scalar=rr[:, 0:1], in1=tmpo[:], op0=ALU.mult, op1=ALU.subtract), reads=[O1, rr, tmpo], writes=[oo])
                    k.op("act", lambda e: e.activation(out=jk[:], in_=oo[:], func=AF.Square, accum_out=ms[:]), reads=[oo], writes=[jk, ms])
                    k.op("dve", lambda e: e.tensor_scalar(out=ms[:], in0=ms[:], scalar1=1.0 / DV, scalar2=1e-5, op0=ALU.mult, op1=ALU.add), reads=[ms], writes=[ms])
                    k.op("act", lambda e: e.activation(out=ms[:], in_=ms[:], func=AF.Sqrt), reads=[ms], writes=[ms])
                    k.op("dve", lambda e: e.reciprocal(out=ms[:], in_=ms[:]), reads=[ms], writes=[ms])
                    k.op("dve", lambda e: e.scalar_tensor_tensor(out=ab[:], in0=oo[:], scalar=ms[:], in1=gsub[:], op0=ALU.mult, op1=ALU.mult), reads=[oo, ms, gsub], writes=[ab])
                    k.op("pe", lambda e: e.transpose(out=pA[:], in_=ab[:], identity=ident[:]), reads=[ab, ident], writes=[pA])
                    cp(k, "act", aTh[hs_][:, j * 128:(j + 1) * 128], pA[:], [pA], [aTh[hs_]])
                k.dma("act", [(catT_d[h], aTh[hs_][:])], aTh[hs_], reads=[aTh[hs_]], writes=[catT_d])
        if "a" in dbg:
            with k.phase():
                o = outp("dbg_aT", [8, 128, NOWN], BF16)
                tmp = k.sb("dbgta", [128, 8, NOWN], BF16)
                k.dma("sp", [(tmp[:], catT_d[0:8].rearrange("h p n -> p h n"))], tmp, reads=[catT_d], writes=[tmp])
                k.dma("sp", [(o.rearrange("h p n -> p h n"), tmp[:])], tmp, reads=[tmp])

        if upto >= 4:
          with k.phase():
            y_tm = [k.sb("y_tm%d" % i, [128, 8, 1024], F32) for i in range(2)]
            wscope = contextlib.ExitStack()
            wphase = k.phase()
            wphase.__enter__()
            PmRe = k.sb("PmRe", [128, 32, 128], BF16)
            PmIm = k.sb("PmIm", [128, 32, 128], BF16)
            Mm = k.sb("Mm", [128, 64, 128], BF16)
            QmRe = k.sb("QmRe", [128, 32, 128], BF16)
            QmIm = k.sb("QmIm", [128, 32, 128], BF16)
            th8 = k.sb("th8", [128, 32], F32)
            dec8 = k.sb("dec8", [128, 32], F32)
            iot = k.sb("iot", [128, 512], F32)
            ioti = k.sb("ioti", [128, 512], I32)
            k.op("pool", lambda e: e.iota(ioti[:], pattern=[[1, 512]], base=0, channel_multiplier=0), writes=[ioti])
            cp(k, "dve", iot[:], ioti[:], [ioti], [iot])
            if True:
              with k.phase():
                    def T(name, shape, dt=F32):
                        return k.sb(name, shape, dt)
                    lre = T("lre", [128, 32]); lim = T("lim", [128, 32]); dtt = T("dtt", [128, 32])
                    with nc.allow_non_contiguous_dma(reason="tiny transposed parameter loads"):
                        for gh in range(2):
                            k.dma("sp", [(lre[gh * 64:(gh + 1) * 64, :], ssm_lam_re[gh * 32:(gh + 1) * 32, :].rearrange("g p -> p g"))], lre, reads=[lre], writes=[lre])
                            k.dma("sp", [(lim[gh * 64:(gh + 1) * 64, :], ssm_lam_im[gh * 32:(gh + 1) * 32, :].rearrange("g p -> p g"))], lim, reads=[lim], writes=[lim])
                            k.dma("sp", [(dtt[gh * 64:(gh + 1) * 64, :], ssm_log_dt[gh * 32:(gh + 1) * 32].partition_broadcast(64))], dtt, reads=[dtt], writes=[dtt])
                        k.flush()
                    k.op("act", lambda e: e.activation(out=dtt[:], in_=dtt[:], func=AF.Exp), reads=[dtt], writes=[dtt])
                    tq = T("tq", [128, 32]); ti = T("ti", [128, 32], I32); tr = T("tr", [128, 32]); tm = T("tm", [128, 32])
                    tmps = (tq, ti, tr, tm)
                    mag = T("mag", [128, 32]); th = T("th", [128, 32]); thc = T("thc", [128, 32])
                    s1 = T("s1", [128, 32]); c1 = T("c1", [128, 32]); ar = T("ar", [128, 32]); ai = T("ai", [128, 32])
                    def tt(out, a, b, op, rd, wr, eng="dve"):
                        k.op(eng, lambda e: e.tensor_tensor(out=out, in0=a, in1=b, op=op), reads=rd, writes=wr)
                    tt(mag[:], lre[:], dtt[:], ALU.mult, [lre, dtt], [mag])
                    k.op("act", lambda e: e.activation(out=dec8[:], in_=mag[:], func=AF.Exp, scale=8.0), reads=[mag], writes=[dec8])
                    k.op("act", lambda e: e.activation(out=mag[:], in_=mag[:], func=AF.Exp), reads=[mag], writes=[mag])
                    tt(th[:], lim[:], dtt[:], ALU.mult, [lim, dtt], [th])
                    k.op("dve", lambda e: e.tensor_scalar(out=thc[:], in0=th[:], scalar1=math.pi / 2, scalar2=None, op0=ALU.add), reads=[th], writes=[thc])
                    sin_red(s1, s1[:], th, th[:], (128, 32), tmps)
                    sin_red(c1, c1[:], thc, thc[:], (128, 32), tmps)
                    tt(ar[:], mag[:], c1[:], ALU.mult, [mag, c1], [ar])
                    tt(ai[:], mag[:], s1[:], ALU.mult, [mag, s1], [ai])
                    th8x = T("th8x", [128, 32])
                    k.op("dve", lambda e: e.tensor_scalar(out=th8x[:], in0=th[:], scalar1=8.0, scalar2=None, op0=ALU.mult), reads=[th], writes=[th8x])
                    jn = T("jn", [128, 32])
                    sin_red(jn, jn[:], th8x, th8x[:], (128, 32), tmps)
                    cp(k, "dve", th8[:], tr[:, 0:32], [tr], [th8])
                    den = T("den", [128, 32]); t1 = T("t1", [128, 32]); t2 = T("t2", [128, 32]); nr = T("nr", [128, 32])
                    cr = T("cr", [128, 32]); ci = T("ci", [128, 32])
                    tt(den[:], lre[:], lre[:], ALU.mult, [lre], [den])
                    tt(t1[:], lim[:], lim[:], ALU.mult, [lim], [t1])
                    tt(den[:], den[:], t1[:], ALU.add, [den, t1], [den])
                    k.op("dve", lambda e: e.reciprocal(out=den[:], in_=den[:]), reads=[den], writes=[den])
                    k.op("dve", lambda e: e.tensor_scalar(out=nr[:], in0=ar[:], scalar1=-1.0, scalar2=None, op0=ALU.add), reads=[ar], writes=[nr])
                    tt(t1[:], nr[:], lre[:], ALU.mult, [nr, lre], [t1])
                    tt(t2[:], ai[:], lim[:], ALU.mult, [ai, lim], [t2])
                    tt(t1[:], t1[:], t2[:], ALU.add, [t1, t2], [t1])
                    tt(cr[:], t1[:], den[:], ALU.mult, [t1, den], [cr])
                    tt(t1[:], ai[:], lre[:], ALU.mult, [ai, lre], [t1])
                    tt(t2[:], nr[:], lim[:], ALU.mult, [nr, lim], [t2])
                    tt(t1[:], t1[:], t2[:], ALU.subtract, [t1, t2], [t1])
                    tt(ci[:], t1[:], den[:], ALU.mult, [t1, den], [ci])
                    ir = T("ir", [128, 32]); ii = T("ii", [128, 32])
                    tt(t1[:], ar[:], ar[:], ALU.mult, [ar], [t1])
                    tt(t2[:], ai[:], ai[:], ALU.mult, [ai], [t2])
                    tt(t1[:], t1[:], t2[:], ALU.add, [t1, t2], [t1])
                    k.op("dve", lambda e: e.reciprocal(out=t1[:], in_=t1[:]), reads=[t1], writes=[t1])
                    tt(ir[:], ar[:], t1[:], ALU.mult, [ar, t1], [ir])
                    tt(ii[:], ai[:], t1[:], ALU.mult, [ai, t1], [ii])
                    k.op("dve", lambda e: e.tensor_scalar(out=ii[:], in0=ii[:], scalar1=-1.0, scalar2=None, op0=ALU.mult), reads=[ii], writes=[ii])
                    pTr = T("pTr", [128, 32, 9]); pTi = T("pTi", [128, 32, 9])
                    pNr = T("pNr", [128, 32, 8]); pNi = T("pNi", [128, 32, 8])
                    pAr = T("pAr", [128, 32, 8]); pAi = T("pAi", [128, 32, 8])
                    for (pr_, pi_) in ((pTr, pTi), (pNr, pNi)):
                        k.op("dve", lambda e, pr_=pr_: e.memset(pr_[:, :, 0:1], 1.0), reads=[pr_], writes=[pr_])
                        k.op("dve", lambda e, pi_=pi_: e.memset(pi_[:, :, 0:1], 0.0), reads=[pi_], writes=[pi_])
                    def cmul(pr_, pi_, kk, mr, mi):
                        tt(t1[:], pr_[:, :, kk], mr[:], ALU.mult, [pr_, mr], [t1])
                        tt(t2[:], pi_[:, :, kk], mi[:], ALU.mult, [pi_, mi], [t2])
                        tt(pr_[:, :, kk + 1], t1[:], t2[:], ALU.subtract, [t1, t2, pr_], [pr_])
                        tt(t1[:], pr_[:, :, kk], mi[:], ALU.mult, [pr_, mi], [t1])
                        tt(t2[:], pi_[:, :, kk], mr[:], ALU.mult, [pi_, mr], [t2])
                        tt(pi_[:, :, kk + 1], t1[:], t2[:], ALU.add, [t1, t2, pi_], [pi_])
                    for kk in range(8):
                        cmul(pTr, pTi, kk, ar, ai)
                    for kk in range(7):
                        cmul(pNr, pNi, kk, ir, ii)
                    for s_ in range(8):
                        cp(k, "dve", pAr[:, :, s_], pTr[:, :, 7 - s_], [pTr, pAr], [pAr])
                        cp(k, "dve", pAi[:, :, s_], pTi[:, :, 7 - s_], [pTi, pAi], [pAi])
                    Bre = T("Bre", [128, 32, 16]); Bim = T("Bim", [128, 32, 16])
                    for gh in range(2):
                        k.dma("sp", [(Bre[gh * 64:(gh + 1) * 64], ssm_b_re[gh * 32:(gh + 1) * 32].rearrange("g p h -> p g h"))], Bre, reads=[Bre], writes=[Bre])
                        k.dma("sp", [(Bim[gh * 64:(gh + 1) * 64], ssm_b_im[gh * 32:(gh + 1) * 32].rearrange("g p h -> p g h"))], Bim, reads=[Bim], writes=[Bim])
                    Bbr = T("Bbr", [128, 32, 16]); Bbi = T("Bbi", [128, 32, 16]); tb1 = T("tb1", [128, 32, 16]); tb2 = T("tb2", [128, 32, 16])
                    def bc16(x):
                        return x[:].unsqueeze(2).to_broadcast([128, 32, 16])
                    tt(tb1[:], Bre[:], bc16(cr), ALU.mult, [Bre, cr], [tb1])
                    tt(tb2[:], Bim[:], bc16(ci), ALU.mult, [Bim, ci], [tb2])
                    tt(Bbr[:], tb1[:], tb2[:], ALU.subtract, [tb1, tb2], [Bbr])
                    tt(tb1[:], Bim[:], bc16(cr), ALU.mult, [Bim, cr], [tb1])
                    tt(tb2[:], Bre[:], bc16(ci), ALU.mult, [Bre, ci], [tb2])
                    tt(Bbi[:], tb1[:], tb2[:], ALU.add, [tb1, tb2], [Bbi])
                    CrT = T("CrT", [128, 32, 16]); CiT = T("CiT", [128, 32, 16])
                    cst = T("cst", [128, 128]); pcs = k.ps("pcs", [128, 128], F32)
                    for (src, dstT) in ((ssm_c_re, CrT), (ssm_c_im, CiT)):
                        for q in range(4):
                            k.dma("sp", [(cst[:, 0:64], src[8 * q:8 * q + 8].rearrange("g h p -> (g h) p")),
                                         (cst[:, 64:128], src[32 + 8 * q:32 + 8 * q + 8].rearrange("g h p -> (g h) p"))], cst, reads=[cst], writes=[cst])
                            k.op("pe", lambda e: e.transpose(out=pcs[:], in_=cst[:], identity=identf[:]), reads=[cst, identf], writes=[pcs])
                            cp(k, "dve", dstT[:, 8 * q:8 * q + 8, :], pcs[:].rearrange("p (g h) -> p g h", h=16), [pcs], [dstT])
                    dcol = T("dcol", [128, 64])
                    with nc.allow_non_contiguous_dma(reason="tiny transposed parameter loads"):
                        for s_ in range(8):
                            k.dma("sp", [(dcol[s_ * 16:(s_ + 1) * 16, :], ssm_d.rearrange("g h -> h g"))], dcol, reads=[dcol], writes=[dcol])
                        k.flush()
                    maskM = T("maskM", [128, 128])
                    k.op("pool", lambda e: e.memset(maskM[:], 1.0), writes=[maskM])
                    k.op("pool", lambda e: e.affine_select(out=maskM[:].rearrange("p (t h) -> p t h", h=16), in_=maskM[:].rearrange("p (t h) -> p t h", h=16), pattern=[[16, 8], [0, 16]],
                                                           compare_op=ALU.is_ge, fill=0.0, base=15, channel_multiplier=-1), reads=[maskM], writes=[maskM])
                    HG = 2
                    B7r = T("B7r", [128, HG, 8, 16]); B7i = T("B7i", [128, HG, 8, 16]); BNr = T("BNr", [128, HG, 8, 16]); BNi = T("BNi", [128, HG, 8, 16])
                    Ctr = T("Ctr", [128, HG, 8, 16]); Cti = T("Cti", [128, HG, 8, 16]); Qr = T("Qr", [128, HG, 8, 16]); Qi = T("Qi", [128, HG, 8, 16])
                    X1 = T("X1", [128, HG, 8, 16]); X2 = T("X2", [128, HG, 8, 16])
                    pM = [k.ps("pM%d" % i, [128, 128], F32) for i in range(2)]
                    pP = [k.ps("pP%d" % i, [128, 128], F32) for i in range(2)]
                    mt1 = T("mt1", [128, 128]); mt2 = T("mt2", [128, 128])
                    for half in range(32 // HG):
                        gs = slice(half * HG, (half + 1) * HG)
                        def pw(tbl, lo, hi):
                            return tbl[:, gs, lo:hi].unsqueeze(3).to_broadcast([128, HG, 8, 16])
                        def vb(tbl):
                            return tbl[:, gs, :].unsqueeze(2).to_broadcast([128, HG, 8, 16])
                        def cprod(outr, outi, pr_, pi_, lo, hi, vr, vi, neg_im, eng="dve"):
                            tt(X1[:], pw(pr_, lo, hi), vb(vr), ALU.mult, [pr_, vr], [X1], eng)
                            tt(X2[:], pw(pi_, lo, hi), vb(vi), ALU.mult, [pi_, vi], [X2], eng)
                            tt(outr[:], X1[:], X2[:], ALU.subtract, [X1, X2], [outr], eng)
                            tt(X1[:], pw(pr_, lo, hi), vb(vi), ALU.mult, [pr_, vi], [X1], eng)
                            tt(X2[:], pw(pi_, lo, hi), vb(vr), ALU.mult, [pi_, vr], [X2], eng)
                            tt(outi[:], X1[:], X2[:], ALU.add, [X1, X2], [outi], eng)
                            if neg_im:
                                k.op(eng, lambda e: e.tensor_scalar(out=outi[:], in0=outi[:], scalar1=-1.0, scalar2=None, op0=ALU.mult), reads=[outi], writes=[outi])
                        cprod(B7r, B7i, pAr, pAi, 0, 8, Bbr, Bbi, False)
                        cprod(BNr, BNi, pNr, pNi, 0, 8, Bbr, Bbi, False)
                        cprod(Ctr, Cti, pTr, pTi, 0, 8, CrT, CiT, True)
                        cprod(Qr, Qi, pTr, pTi, 1, 9, CrT, CiT, True)
                        cp(k, "dve", QmRe[:, gs, :], Qr[:].rearrange("p g t h -> p g (t h)"), [Qr], [QmRe])
                        cp(k, "dve", QmIm[:, gs, :], Qi[:].rearrange("p g t h -> p g (t h)"), [Qi], [QmIm])
                        for gl in range(HG):
                            gi = half * HG + gl
                            for (srcB, dstP, pp_) in ((B7r, PmRe, pP[0]), (B7i, PmIm, pP[1])):
                                k.op("pe", lambda e, srcB=srcB, pp_=pp_, gl=gl: e.transpose(out=pp_[:], in_=srcB[:, gl].rearrange("p s h -> p (s h)"), identity=identf[:]), reads=[srcB, identf], writes=[pp_])
                                cp(k, "act", dstP[:, gi, :], pp_[:], [pp_], [dstP])
                            for gh in range(2):
                                g = gh * 32 + gi
                                pm_ = pM[gh]
                                rs = slice(gh * 64, (gh + 1) * 64)
                                k.op("pe", lambda e, pm_=pm_, rs=rs, gl=gl: e.matmul(pm_[:], lhsT=BNr[rs, gl].rearrange("p s h -> p (s h)"), rhs=Ctr[rs, gl].rearrange("p s h -> p (s h)"), start=True, stop=False), reads=[BNr, Ctr], writes=[pm_], inc=False)
                                k.op("pe", lambda e, pm_=pm_, rs=rs, gl=gl: e.matmul(pm_[:], lhsT=BNi[rs, gl].rearrange("p s h -> p (s h)"), rhs=Cti[rs, gl].rearrange("p s h -> p (s h)"), start=False, stop=True), reads=[BNi, Cti], writes=[pm_])
                                tt(mt1[:], pm_[:], maskM[:], ALU.mult, [pm_, maskM], [mt1])
                                k.op("dve", lambda e, g=g: e.scalar_tensor_tensor(out=mt2[:], in0=identf[:], scalar=dcol[:, g:g + 1], in1=mt1[:], op0=ALU.mult, op1=ALU.add), reads=[identf, dcol, mt1], writes=[mt2])
                                cp(k, "dve", Mm[:, g, :], mt2[:], [mt2], [Mm])

            with k.phase():
                tq = k.sb("tq", [128, 512], F32); ti = k.sb("ti", [128, 512], I32); tr = k.sb("tr", [128, 512], F32); tm = k.sb("tm", [128, 512], F32)
                tmps = (tq, ti, tr, tm)
                ang = k.sb("ang", [128, 512], F32); angc = k.sb("angc", [128, 512], F32)
                cosT = k.sb("cosT", [128, 512], F32); sinT = k.sb("sinT", [128, 512], F32)
                u2 = [k.sb("u2_%d" % i, [128, 2, 512], BF16) for i in range(2)]
                Vre = k.ps("Vre", [128, 512], F32); Vim = k.ps("Vim", [128, 512], F32)
                Yp = [k.ps("Yp%d" % i, [128, 4, 128], F32) for i in range(2)]
                a1 = k.sb("a1", [128, 512], F32); a2 = k.sb("a2", [128, 512], F32); a3 = k.sb("a3", [128, 512], F32); a4 = k.sb("a4", [128, 512], F32)
                wri = k.sb("wri", [128, 512], F32); wii = k.sb("wii", [128, 512], F32)
                wr_ = k.sb("wr_", [128, 512], F32); wi_ = k.sb("wi_", [128, 512], F32)
                Xr = [k.sb("Xr%d" % i, [128, 520], BF16) for i in range(2)]
                Xi = [k.sb("Xi%d" % i, [128, 520], BF16) for i in range(2)]
                uo = k.sb("uo", [128, 2, 256], BF16); xro = k.sb("xro", [128, 256], BF16); xio = k.sb("xio", [128, 256], BF16)
                for i in range(2):
                    k.op("pool", lambda e, i=i: e.memset(Xr[i][:], 0.0), writes=[Xr[i]])
                    k.op("pool", lambda e, i=i: e.memset(Xi[i][:], 0.0), writes=[Xi[i]])
                def tt(out, a, b, op, rd, wr, eng="dve"):
                    k.op(eng, lambda e: e.tensor_tensor(out=out, in0=a, in1=b, op=op), reads=rd, writes=wr)
                for gi in range(32):
                    sl_ = gi % 2
                    uu = u2[sl_]
                    k.dma("sp", [(uu[:, gh, :], U_d[:, :, gh * 32 + gi, :].rearrange("s h c -> (s h) c")) for gh in range(2)], uu, reads=[U_d], writes=[uu])
                    k.op("dve", lambda e, gi=gi: e.tensor_scalar(out=ang[:], in0=iot[:], scalar1=th8[:, gi:gi + 1], scalar2=None, op0=ALU.mult), reads=[iot, th8], writes=[ang])
                    k.op("pool", lambda e: e.tensor_scalar(out=angc[:], in0=ang[:], scalar1=math.pi / 2, scalar2=None, op0=ALU.add), reads=[ang], writes=[angc])
                    sin_red(sinT, sinT[:], ang, ang[:], (128, 512), tmps)
                    sin_red(cosT, cosT[:], angc, angc[:], (128, 512), tmps)
                    for gh in range(2):
                        rs = slice(gh * 64, (gh + 1) * 64)
                        k.op("pe", lambda e, rs=rs, gi=gi, gh=gh, uu=uu: e.matmul(Vre[rs, :], lhsT=PmRe[:, gi, rs], rhs=uu[:, gh, :], start=True, stop=True), reads=[PmRe, uu], writes=[Vre], inc=(gh == 1))
                    for gh in range(2):
                        rs = slice(gh * 64, (gh + 1) * 64)
                        k.op("pe", lambda e, rs=rs, gi=gi, gh=gh, uu=uu: e.matmul(Vim[rs, :], lhsT=PmIm[:, gi, rs], rhs=uu[:, gh, :], start=True, stop=True), reads=[PmIm, uu], writes=[Vim], inc=(gh == 1))
                    tt(a1[:], Vre[:], cosT[:], ALU.mult, [Vre, cosT], [a1])
                    tt(a2[:], Vim[:], sinT[:], ALU.mult, [Vim, sinT], [a2])
                    tt(a3[:], Vim[:], cosT[:], ALU.mult, [Vim, cosT], [a3])
                    tt(a4[:], Vre[:], sinT[:], ALU.mult, [Vre, sinT], [a4])
                    tt(wri[:], a1[:], a2[:], ALU.add, [a1, a2], [wri], "pool")
                    tt(wii[:], a3[:], a4[:], ALU.subtract, [a3, a4], [wii], "pool")
                    dbc = dec8[:, gi:gi + 1].to_broadcast([128, 512])
                    k.op("dve", lambda e, dbc=dbc: e.tensor_tensor_scan(out=wr_[:], data0=dbc, data1=wri[:], initial=0.0, op0=ALU.mult, op1=ALU.add), reads=[dec8, wri], writes=[wr_])
                    k.op("dve", lambda e, dbc=dbc: e.tensor_tensor_scan(out=wi_[:], data0=dbc, data1=wii[:], initial=0.0, op0=ALU.mult, op1=ALU.add), reads=[dec8, wii], writes=[wi_])
                    xr_, xi_ = Xr[sl_], Xi[sl_]
                    tt(a1[:], wr_[:], cosT[:], ALU.mult, [wr_, cosT], [a1], "pool")
                    tt(a2[:], wi_[:], sinT[:], ALU.mult, [wi_, sinT], [a2], "pool")
                    tt(xr_[:, 1:513], a1[:], a2[:], ALU.subtract, [a1, a2], [xr_], "pool")
                    tt(a3[:], wi_[:], cosT[:], ALU.mult, [wi_, cosT], [a3], "pool")
                    tt(a4[:], wr_[:], sinT[:], ALU.mult, [wr_, sinT], [a4], "pool")
                    tt(xi_[:, 1:513], a3[:], a4[:], ALU.add, [a3, a4], [xi_], "pool")
                    def sel(dst, dstb, src_ap, srcb, eng1, eng2):
                        v = src_ap
                        k.op(eng1, lambda e: e.tensor_scalar(out=dst, in0=v[0], scalar1=ccs[:, 0:1], scalar2=None, op0=ALU.mult), reads=[srcb, ccs], writes=[dstb])
                        k.op(eng2, lambda e: e.scalar_tensor_tensor(out=dst, in0=v[1], scalar=ccs[:, 1:2], in1=dst, op0=ALU.mult, op1=ALU.add), reads=[srcb, ccs, dstb], writes=[dstb])
                    uv = uu[:].rearrange("p g (j r i) -> p g j r i", r=2, i=16)
                    sel(uo[:].rearrange("p g (j i) -> p g j i", i=16), uo, (uv[:, :, :, 0, :], uv[:, :, :, 1, :]), uu, "pool", "dve")
                    xv = xr_[:, 0:512].rearrange("p (j r i) -> p j r i", r=2, i=16)
                    sel(xro[:].rearrange("p (j i) -> p j i", i=16), xro, (xv[:, :, 0, :], xv[:, :, 1, :]), xr_, "pool", "dve")
                    xv2 = xi_[:, 0:512].rearrange("p (j r i) -> p j r i", r=2, i=16)
                    sel(xio[:].rearrange("p (j i) -> p j i", i=16), xio, (xv2[:, :, 0, :], xv2[:, :, 1, :]), xi_, "pool", "dve")
                    yp = Yp[gi % 2]
                    for gh in range(2):
                        g = gh * 32 + gi
                        rs = slice(gh * 64, (gh + 1) * 64)
                        for cbk in range(2):
                            slot = gh * 2 + cbk
                            cs = slice(cbk * 128, (cbk + 1) * 128)
                            k.op("pe", lambda e, yp=yp, slot=slot, gh=gh, cs=cs, g=g: e.matmul(yp[:, slot, :], lhsT=uo[:, gh, cs], rhs=Mm[:, g, :], start=True, stop=False), reads=[uo, Mm], writes=[yp], inc=False)
                            k.op("pe", lambda e, yp=yp, slot=slot, rs=rs, cs=cs, gi=gi: e.matmul(yp[:, slot, :], lhsT=xro[rs, cs], rhs=QmRe[rs, gi, :], start=False, stop=False), reads=[xro, QmRe], writes=[yp], inc=False)
                            k.op("pe", lambda e, yp=yp, slot=slot, rs=rs, cs=cs, gi=gi: e.matmul(yp[:, slot, :], lhsT=xio[rs, cs], rhs=QmIm[rs, gi, :], start=False, stop=True), reads=[xio, QmIm], writes=[yp], inc=(slot == 3))
                    for gh in range(2):
                        g = gh * 32 + gi
                        for cbk in range(2):
                            slot = gh * 2 + cbk
                            k.op("act", lambda e, yp=yp, slot=slot, g=g, cbk=cbk: e.copy(out=y_tm[cbk][:, :, g * 16:(g + 1) * 16], in_=yp[:, slot, :].rearrange("p (t h) -> p t h", h=16)),
                                 reads=[yp], writes=[y_tm[cbk]])
            wphase.__exit__(None, None, None)
            if "y" in dbg:
                o = outp("dbg_y", [2, 128, 8 * 1024], F32)
                for cbk in range(2):
                    k.dma("sp", [(o[cbk], y_tm[cbk][:].rearrange("p t c -> p (t c)"))], y_tm[cbk], reads=[y_tm[cbk]])
            with k.phase():
                wg = k.sb("wg", [128, 8, 1024], BF16)
                k.dma("pool", [(wg[:], w_glu.rearrange("(kc p) n -> p kc n", p=128))], wg, writes=[wg])
                bg = k.sb("bg", [128, 1024], F32)
                k.dma("sp", [(bg[:], b_glu.partition_broadcast(128))], bg, writes=[bg])
                gn = k.sb("gn", [128, 1024], F32)
                k.dma("sp", [(gn[:], ssm_norm_g.partition_broadcast(128))], gn, writes=[gn])
                x2 = k.sb("x2", [128, 1024], F32); inn = k.sb("inn", [128, 1024], F32); sg_ = k.sb("sg_", [128, 1024], F32)
                sf = k.sb("sf", [128, 1024], F32); sbf = k.sb("sbf", [128, 1024], BF16)
                sT = k.sb("sT", [128, 8, 128], BF16)
                gt = k.sb("gt", [128, 1024], F32); s2 = k.sb("s2", [128, 1024], F32); s3 = k.sb("s3", [128, 1024], BF16)
                jk2 = k.sb("jk2", [128, 1024], BF16); ms2 = k.sb("ms2", [128, 1], F32)
                catS = k.sb("catS", [128, 8, 1024], BF16)
                pTs = k.ps("pTs", [128, 8, 128], BF16)
                pG = [k.ps("pG%d" % i, [128, 512], F32) for i in range(2)]
                C0 = 2.0 * math.sqrt(2.0 / math.pi)
                for cbk in range(2):
                    for t in range(8):
                        yv = y_tm[cbk][:, t, :]
                        k.op("pool", lambda e, yv=yv: e.tensor_tensor(out=x2[:], in0=yv, in1=yv, op=ALU.mult), reads=[y_tm[cbk]], writes=[x2])
                        k.op("dve", lambda e: e.tensor_scalar(out=x2[:], in0=x2[:], scalar1=0.044715, scalar2=1.0, op0=ALU.mult, op1=ALU.add), reads=[x2], writes=[x2])
                        k.op("pool", lambda e, yv=yv: e.tensor_tensor(out=inn[:], in0=x2[:], in1=yv, op=ALU.mult), reads=[x2, y_tm[cbk]], writes=[inn])
                        k.op("act", lambda e: e.activation(out=sg_[:], in_=inn[:], func=AF.Sigmoid, scale=C0), reads=[inn], writes=[sg_])
                        k.op("dve", lambda e, yv=yv: e.tensor_tensor(out=sf[:], in0=sg_[:], in1=yv, op=ALU.mult), reads=[sg_, y_tm[cbk]], writes=[sf])
                        cp(k, "pool", sbf[:], sf[:], [sf], [sbf])
                        for kc in range(8):
                            k.op("pe", lambda e, kc=kc: e.transpose(out=pTs[:, kc, :], in_=sbf[:, kc * 128:(kc + 1) * 128], identity=ident[:]), reads=[sbf, ident], writes=[pTs], inc=(kc == 7))
                        cp(k, "act", sT[:], pTs[:], [pTs], [sT])
                        for half in range(2):
                            for kc in range(8):
                                k.op("pe", lambda e, half=half, kc=kc: e.matmul(pG[half][:], lhsT=sT[:, kc, :], rhs=wg[:, kc, half * 512:(half + 1) * 512], start=(kc == 0), stop=(kc == 7)),
                                     reads=[sT, wg], writes=[pG[half]], inc=(kc == 7))
                            k.op("dve", lambda e, half=half: e.tensor_tensor(out=gt[:, half * 512:(half + 1) * 512], in0=pG[half][:], in1=bg[:, half * 512:(half + 1) * 512], op=ALU.add), reads=[pG[half], bg], writes=[gt])
                        k.op("act", lambda e: e.activation(out=gt[:], in_=gt[:], func=AF.Sigmoid), reads=[gt], writes=[gt])
                        k.op("dve", lambda e: e.tensor_tensor(out=s2[:], in0=gt[:], in1=sf[:], op=ALU.mult), reads=[gt, sf], writes=[s2])
                        k.op("act", lambda e: e.activation(out=jk2[:], in_=s2[:], func=AF.Square, accum_out=ms2[:]), reads=[s2], writes=[jk2, ms2])
                        k.op("dve", lambda e: e.tensor_scalar(out=ms2[:], in0=ms2[:], scalar1=1.0 / 1024, scalar2=EPS, op0=ALU.mult, op1=ALU.add), reads=[ms2], writes=[ms2])
                        k.op("act", lambda e: e.activation(out=ms2[:], in_=ms2[:], func=AF.Sqrt), reads=[ms2], writes=[ms2])
                        k.op("dve", lambda e: e.reciprocal(out=ms2[:], in_=ms2[:]), reads=[ms2], writes=[ms2])
                        k.op("dve", lambda e: e.scalar_tensor_tensor(out=s3[:], in0=s2[:], scalar=ms2[:], in1=gn[:], op0=ALU.mult, op1=ALU.mult), reads=[s2, ms2, gn], writes=[s3])
                        for kc in range(8):
                            k.op("pe", lambda e, kc=kc: e.transpose(out=pTs[:, kc, :], in_=s3[:, kc * 128:(kc + 1) * 128], identity=ident[:]), reads=[s3, ident], writes=[pTs], inc=(kc == 7))
                        cp(k, "act", catS[:, :, t * 128:(t + 1) * 128], pTs[:], [pTs], [catS])
                    k.dma("sp", [(catT_d[8:16, :, cbk * 1024:(cbk + 1) * 1024].rearrange("c p n -> p c n"), catS[:])], catS, reads=[catS], writes=[catT_d])
        if "s" in dbg:
            with k.phase():
                o = outp("dbg_sT", [8, 128, NOWN], BF16)
                tmp = k.sb("dbgts", [128, 8, NOWN], BF16)
                k.dma("sp", [(tmp[:], catT_d[8:16].rearrange("h p n -> p h n"))], tmp, reads=[catT_d], writes=[tmp])
                k.dma("sp", [(o.rearrange("h p n -> p h n"), tmp[:])], tmp, reads=[tmp])

        if upto >= 5:
          with k.phase():
            wo = k.sb("wo", [128, 16, D], BF16)
            w_ov = w_o.rearrange("(kc p) n -> p kc n", p=128)
            k.dma("pool", [(wo[:, 4 * q:4 * q + 4, :], w_ov[:, 4 * q:4 * q + 4, :]) for q in range(4)], wo, writes=[wo])
            gf = k.sb("gf", [128, D], F32)
            k.dma("sp", [(gf[:], g_ffn.partition_broadcast(128))], gf, writes=[gf])
            wrs = k.sb("wrs", [128, 16, 36], F32)
            k.dma("sp", [(wrs[:], wr.rearrange("(kc p) n -> p kc n", p=128))], wrs, writes=[wrs])
            brb = k.sb("brb", [128, 36], F32)
            k.dma("sp", [(brb[:], br.partition_broadcast(128))], brb, writes=[brb])
            Ltri = k.sb("Ltri", [128, 128], BF16); onesb = k.sb("onesb", [128, 128], BF16); ltf = k.sb("ltf", [128, 128], F32)
            k.op("pool", lambda e: e.memset(ltf[:], 1.0), writes=[ltf])
            k.op("pool", lambda e: e.affine_select(out=ltf[:], in_=ltf[:], pattern=[[1, 128]], compare_op=ALU.is_ge, fill=0.0, base=-1, channel_multiplier=-1), reads=[ltf], writes=[ltf])
            cp(k, "dve", Ltri[:], ltf[:], [ltf], [Ltri])
            k.op("pool", lambda e: e.memset(onesb[:], 1.0), writes=[onesb])
            iote = k.sb("iote", [128, 32], F32); iotei = k.sb("iotei", [128, 32], I32)
            k.op("pool", lambda e: e.iota(iotei[:], pattern=[[1, 32]], base=0, channel_multiplier=0), writes=[iotei])
            cp(k, "dve", iote[:], iotei[:], [iotei], [iote])
            cntb = k.sb("cntb", [128, 32], F32)
            k.op("dve", lambda e: e.memset(cntb[:], 0.0), writes=[cntb])
            aTs = k.sb("aTs", [128, 8, 1024], BF16); sTs = k.sb("sTs", [128, 8, 1024], BF16)
            xot = [k.sb("xot%d" % i, [128, D], F32) for i in range(2)]
            x1t = [k.sb("x1t%d" % i, [128, D], F32) for i in range(2)]
            h2f = k.sb("h2f", [128, D], F32); h2b = [k.sb("h2b%d" % i, [128, D], BF16) for i in range(2)]
            jk5 = k.sb("jk5", [128, D], BF16); ms5 = k.sb("ms5", [128, 1], F32)
            h2T = k.sb("h2T", [128, 16, 128], F32)
            pO = [k.ps("pO%d" % i, [128, 512], F32) for i in range(4)]
            pX = [k.ps("pX%d" % i, [128, 4, 128], F32) for i in range(2)]
            pR = k.ps("pR", [128, 512], F32)
            def S_(name, n, dt=F32):
                return k.sb(name, [128, n], dt)
            Lg = S_("Lg", 36); mg = S_("mg", 1); nmg = S_("nmg", 1); ohg = S_("ohg", 4); eg = S_("eg", 4); zg = S_("zg", 1); gate = S_("gate", 1)
            tm3 = k.sb("tm3", [128, 8, 4], F32); les = S_("les", 8); m1 = S_("m1", 1); oh1 = S_("oh1", 8); les2 = S_("les2", 8); m2 = S_("m2", 1); oh2 = S_("oh2", 8)
            dd = S_("dd", 1); e2 = S_("e2", 1); rden = S_("rden", 1)
            OH = [k.sb("OH%d" % i, [128, 4, 8], F32) for i in range(2)]
            posk = k.sb("posk", [128, 16, 2], F32); eidk = k.sb("eidk", [128, 16, 2], F32)
            Ab = S_("Ab", 32, BF16); posf = S_("posf", 32); t32 = S_("t32", 32); pk = S_("pk", 1); ek = S_("ek", 1); ov = S_("ov", 1); df = S_("df", 1)
            def ts(out, a, s1, s2, o0, o1, rd, wr_):
                k.op("dve", lambda e: e.tensor_scalar(out=out, in0=a, scalar1=s1, scalar2=s2, op0=o0, **({"op1": o1} if o1 is not None else {})), reads=rd, writes=wr_)
            def tt(out, a, b, op, rd, wr_, eng="dve"):
                k.op(eng, lambda e: e.tensor_tensor(out=out, in0=a, in1=b, op=op), reads=rd, writes=wr_)
            def red(out, a, op, rd, wr_):
                k.op("dve", lambda e: e.tensor_reduce(out=out, in_=a, axis=AX.X, op=op), reads=rd, writes=wr_)
            for it in range(16):
                cbk, t = it // 8, it % 8
                if t == 0:
                    k.dma("sp", [(aTs[:], catT_d[0:8, :, cbk * 1024:(cbk + 1) * 1024].rearrange("c p n -> p c n"))], aTs, reads=[catT_d], writes=[aTs])
                    k.dma("sp", [(sTs[:], catT_d[8:16, :, cbk * 1024:(cbk + 1) * 1024].rearrange("c p n -> p c n"))], sTs, reads=[catT_d], writes=[sTs])
                xs = xot[it % 2]; x1 = x1t[it % 2]; hb_ = h2b[it % 2]
                k.dma("act", [(xs[:], xo[it * 128:(it + 1) * 128, :])], xs, writes=[xs])
                aview = aTs[:].rearrange("p h (c t) -> p h c t", t=8)
                for nb in range(4):
                    pp = pO[nb]
                    for kc in range(16):
                        lhs = aview[:, kc, :, t] if kc < 8 else sTs[:, kc - 8, t * 128:(t + 1) * 128]
                        rdb = aTs if kc < 8 else sTs
                        k.op("pe", lambda e, pp=pp, lhs=lhs, kc=kc, nb=nb: e.matmul(pp[:], lhsT=lhs, rhs=wo[:, kc, nb * 512:(nb + 1) * 512], start=(kc == 0), stop=(kc == 15)),
                             reads=[rdb, wo], writes=[pp], inc=(kc == 15))
                    tt(x1[:, nb * 512:(nb + 1) * 512], pp[:], xs[:, nb * 512:(nb + 1) * 512], ALU.add, [pp, xs], [x1])
                k.dma("act", [(X1_d[it * 128:(it + 1) * 128, :], x1[:])], x1, reads=[x1], writes=[X1_d])
                k.op("act", lambda e, x1=x1: e.activation(out=jk5[:], in_=x1[:], func=AF.Square, accum_out=ms5[:]), reads=[x1], writes=[jk5, ms5])
                ts(ms5[:], ms5[:], 1.0 / D, EPS, ALU.mult, ALU.add, [ms5], [ms5])
                k.op("act", lambda e: e.activation(out=ms5[:], in_=ms5[:], func=AF.Sqrt), reads=[ms5], writes=[ms5])
                k.op("dve", lambda e: e.reciprocal(out=ms5[:], in_=ms5[:]), reads=[ms5], writes=[ms5])
                k.op("dve", lambda e, x1=x1: e.scalar_tensor_tensor(out=h2f[:], in0=x1[:], scalar=ms5[:], in1=gf[:], op0=ALU.mult, op1=ALU.mult), reads=[x1, ms5, gf], writes=[h2f])
                cp(k, "pool", hb_[:], h2f[:], [h2f], [hb_])
                for q in range(4):
                    px = pX[q % 2]
                    for c in range(4):
                        kc = q * 4 + c
                        k.op("pe", lambda e, px=px, c=c, kc=kc: e.transpose(out=px[:, c, :], in_=h2f[:, kc * 128:(kc + 1) * 128], identity=identf[:]), reads=[h2f, identf], writes=[px], inc=(c == 3))
                    cp(k, "act", h2T[:, q * 4:(q + 1) * 4, :], px[:], [px], [h2T])
                for kc in range(16):
                    k.op("pe", lambda e, kc=kc: e.matmul(pR[:, 0:36], lhsT=h2T[:, kc, :], rhs=wrs[:, kc, :], start=(kc == 0), stop=(kc == 15)), reads=[h2T, wrs], writes=[pR], inc=(kc == 15))
                tt(Lg[:], pR[:, 0:36], brb[:], ALU.add, [pR, brb], [Lg])
                red(mg[:], Lg[:, 0:4], ALU.max, [Lg], [mg])
                ts(ohg[:], Lg[:, 0:4], mg[:, 0:1], None, ALU.is_equal, None, [Lg, mg], [ohg])
                ts(nmg[:], mg[:], -1.0, None, ALU.mult, None, [mg], [nmg])
                k.op("act", lambda e: e.activation(out=eg[:], in_=Lg[:, 0:4], func=AF.Exp, bias=nmg[:], scale=1.0, accum_out=zg[:]), reads=[Lg, nmg], writes=[eg, zg])
                k.op("dve", lambda e: e.reciprocal(out=gate[:], in_=zg[:]), reads=[zg], writes=[gate])
                tt(tm3[:], Lg[:, 4:36].rearrange("p (g e) -> p e g", g=4), ohg[:].unsqueeze(1).to_broadcast([128, 8, 4]), ALU.mult, [Lg, ohg], [tm3])
                red(les[:], tm3[:], ALU.add, [tm3], [les])
                red(m1[:], les[:], ALU.max, [les], [m1])
                ts(oh1[:], les[:], m1[:, 0:1], None, ALU.is_equal, None, [les, m1], [oh1])
                k.op("dve", lambda e: e.scalar_tensor_tensor(out=les2[:], in0=oh1[:], scalar=-1e30, in1=les[:], op0=ALU.mult, op1=ALU.add), reads=[oh1, les], writes=[les2])
                red(m2[:], les2[:], ALU.max, [les2], [m2])
                ts(oh2[:], les2[:], m2[:, 0:1], None, ALU.is_equal, None, [les2, m2], [oh2])
                tt(dd[:], m2[:], m1[:], ALU.subtract, [m2, m1], [dd])
                k.op("act", lambda e: e.activation(out=e2[:], in_=dd[:], func=AF.Exp), reads=[dd], writes=[e2])
                ts(rden[:], e2[:], 1.0, None, ALU.add, None, [e2], [rden])
                k.op("dve", lambda e: e.reciprocal(out=rden[:], in_=rden[:]), reads=[rden], writes=[rden])
                tt(wts[:, it, 0:1], gate[:], rden[:], ALU.mult, [gate, rden, wts], [wts])
                tt(wts[:, it, 1:2], wts[:, it, 0:1], e2[:], ALU.mult, [wts, e2], [wts])
                for kk, ohk in enumerate((oh1, oh2)):
                    tt(OH[kk][:], ohg[:].unsqueeze(2).to_broadcast([128, 4, 8]), ohk[:].unsqueeze(1).to_broadcast([128, 4, 8]), ALU.mult, [ohg, ohk], [OH[kk]])
                tt(Ab[:], OH[0][:].rearrange("p g e -> p (g e)"), OH[1][:].rearrange("p g e -> p (g e)"), ALU.add, [OH[0], OH[1]], [Ab])
                k.op("pe", lambda e: e.matmul(pR[:, 64:96], lhsT=Ltri[:], rhs=Ab[:], start=True, stop=True), reads=[Ltri, Ab], writes=[pR], inc=False)
                k.op("pe", lambda e: e.matmul(pR[:, 128:160], lhsT=onesb[:], rhs=Ab[:], start=True, stop=True), reads=[onesb, Ab], writes=[pR])
                tt(posf[:], pR[:, 64:96], cntb[:], ALU.add, [pR, cntb], [posf])
                tt(cntb[:], pR[:, 128:160], cntb[:], ALU.add, [pR, cntb], [cntb])
                for kk in range(2):
                    ohf = OH[kk][:].rearrange("p g e -> p (g e)")
                    tt(t32[:], ohf, posf[:], ALU.mult, [OH[kk], posf], [t32])
                    red(pk[:], t32[:], ALU.add, [t32], [pk])
                    tt(t32[:], ohf, iote[:], ALU.mult, [OH[kk], iote], [t32])
                    red(ek[:], t32[:], ALU.add, [t32], [ek])
                    cp(k, "dve", posk[:, it, kk:kk + 1], pk[:], [pk, posk], [posk])
                    cp(k, "dve", eidk[:, it, kk:kk + 1], ek[:], [ek, eidk], [eidk])
                k.dma("act", [(H2_d[it * 128:(it + 1) * 128, :], hb_[:])], hb_, reads=[hb_], writes=[H2_d])
            t3i = k.sb("t3i", [128, 32, 32], I32); t3a = k.sb("t3a", [128, 32, 32], F32); t3b = k.sb("t3b", [128, 32, 32], F32)
            k.op("pool", lambda e: e.iota(t3i[:], pattern=[[0, 32], [128, 32]], base=0, channel_multiplier=0), writes=[t3i])
            cp(k, "dve", t3a[:], t3i[:], [t3i], [t3a])
            nblk = S_("nblk", 32); cumb = S_("cumb", 32); pstart = S_("pstart", 32); ones32 = S_("ones32", 32)
            tt(t3b[:], cntb[:].unsqueeze(2).to_broadcast([128, 32, 32]), t3a[:], ALU.is_gt, [cntb, t3a], [t3b])
            red(nblk[:], t3b[:], ALU.add, [t3b], [nblk])
            k.op("dve", lambda e: e.memset(ones32[:], 1.0), writes=[ones32])
            k.op("dve", lambda e: e.tensor_tensor_scan(out=cumb[:], data0=ones32[:], data1=nblk[:], initial=0.0, op0=ALU.mult, op1=ALU.add), reads=[ones32, nblk], writes=[cumb])
            tt(pstart[:], cumb[:], nblk[:], ALU.subtract, [cumb, nblk], [pstart])
            ts(pstart[:], pstart[:], 128.0, None, ALU.mult, None, [pstart], [pstart])
            bsi = k.sb("bsi", [128, NE, 4], I32); bsf = k.sb("bsf", [128, NE, 4], F32); vld = k.sb("vld", [128, NE, 4], F32); jv = k.sb("jv", [128, NE, 4], F32)
            k.op("pool", lambda e: e.iota(bsi[:], pattern=[[0, NE], [128, 4]], base=0, channel_multiplier=1), writes=[bsi])
            cp(k, "dve", bsf[:], bsi[:], [bsi], [bsf])
            tt(bsf[:], bsf[:], pstart[:].unsqueeze(2).to_broadcast([128, NE, 4]), ALU.add, [bsf, pstart], [bsf])
            k.op("pool", lambda e: e.iota(bsi[:], pattern=[[0, NE], [1, 4]], base=0, channel_multiplier=0), reads=[bsi], writes=[bsi])
            cp(k, "dve", jv[:], bsi[:], [bsi], [jv])
            tt(vld[:], nblk[:].unsqueeze(2).to_broadcast([128, NE, 4]), jv[:], ALU.is_le, [nblk, jv], [vld])
            k.op("dve", lambda e: e.scalar_tensor_tensor(out=vld[:], in0=vld[:], scalar=1.0e6, in1=bsf[:], op0=ALU.mult, op1=ALU.add), reads=[vld, bsf], writes=[vld])
            cp(k, "dve", idxout[:], vld[:], [vld], [idxout])
            ts(bsf[:], bsf[:], float(NROW - 1), None, ALU.min, None, [bsf], [bsf])
            cp(k, "dve", idxin[:], bsf[:], [bsf], [idxin])
            for it in range(16):
                hb_ = h2b[it % 2]
                for kk in range(2):
                    ts(t32[:], iote[:], eidk[:, it, kk:kk + 1], None, ALU.is_equal, None, [iote, eidk], [t32])
                    tt(t32[:], t32[:], pstart[:], ALU.mult, [t32, pstart], [t32])
                    red(df[:], t32[:], ALU.add, [t32], [df])
                    tt(df[:], df[:], posk[:, it, kk:kk + 1], ALU.add, [df, posk], [df])
                    cp(k, "dve", dest_i[:, it, kk:kk + 1], df[:], [df, dest_i], [dest_i])
                k.dma("sp", [(hb_[:], H2_d[it * 128:(it + 1) * 128, :])], hb_, reads=[H2_d], writes=[hb_])
                k.dma_fn("pool", [(lambda e, kk=kk, hb_=hb_, it=it: e.indirect_dma_start(out=H_d[:, :], out_offset=bass.IndirectOffsetOnAxis(ap=dest_i[:, it, kk:kk + 1], axis=0),
                                                                                in_=hb_[:, :], in_offset=None, bounds_check=bcreg(e), oob_is_err=False)) for kk in range(2)],
                         hb_, reads=[hb_, dest_i], writes=[H_d])
            if "r" in dbg:
                k.dma("sp", [(outp("dbg_dest", [128, 32], I32), dest_i[:].rearrange("p a b -> p (a b)"))], dest_i, reads=[dest_i])
                k.dma("sp", [(outp("dbg_wts", [128, 32]), wts[:].rearrange("p a b -> p (a b)"))], wts, reads=[wts])
                k.dma("sp", [(outp("dbg_cnt", [128, 32]), cntb[:])], cntb, reads=[cntb])
                k.dma("sp", [(outp("dbg_idxin", [128, NE * 4], I32), idxin[:].rearrange("p a b -> p (a b)"))], idxin, reads=[idxin])
                k.dma("sp", [(outp("dbg_idxout", [128, NE * 4], I32), idxout[:].rearrange("p a b -> p (a b)"))], idxout, reads=[idxout])
        if "x1" in dbg:
            with k.phase():
                o = outp("dbg_x1", [NOWN, D], F32)
                tmp = k.sb("dbgtx", [128, 16, D], F32)
                k.dma("sp", [(tmp[:], X1_d[:].rearrange("(t p) n -> p t n", p=128))], tmp, reads=[X1_d], writes=[tmp])
                k.dma("sp", [(o.rearrange("(t p) n -> p t n", p=128), tmp[:])], tmp, reads=[tmp])

        if upto >= 6 and "skip6" not in dbg:
          with k.phase():
            W1s = [k.sb("W1s%d" % i, [128, 16, DE], BF16) for i in range(2)]
            W3s = [k.sb("W3s%d" % i, [128, 16, DE], BF16) for i in range(2)]
            W2s = [k.sb("W2s%d" % i, [128, 4, D], BF16) for i in range(2)]
            xin = [k.sb("xin%d" % i, [128, D], BF16) for i in range(2)]
            xT = [k.sb("xT%d" % i, [128, 16, 128], BF16) for i in range(2)]
            slu = k.sb("slu", [128, 512], F32); gT = k.sb("gT", [128, 4, 128], BF16)
            Yt = [[k.sb("Yt%d_%d" % (i, hh), [128, D // 2], F32) for hh in range(2)] for i in range(2)]
            pXT = [k.ps("pXT%d" % i, [128, 8, 128], BF16) for i in range(2)]
            pH1 = k.ps("pH1", [128, 4, 128], F32); pH3 = k.ps("pH3", [128, 4, 128], F32)
            pYo = [k.ps("pYo%d" % i, [128, 512], F32) for i in range(4)]
            nb_ = 0
            for ex in range(NE):
                ws_ = ex % 2
                k.dma("pool", [(W1s[ws_][:, 0:8, :], w1[ex].rearrange("(kc p) n -> p kc n", p=128)[:, 0:8, :]), (W1s[ws_][:, 8:16, :], w1[ex].rearrange("(kc p) n -> p kc n", p=128)[:, 8:16, :])], W1s[ws_], writes=[W1s[ws_]])
                k.dma("pool", [(W3s[ws_][:, 0:8, :], w3[ex].rearrange("(kc p) n -> p kc n", p=128)[:, 0:8, :]), (W3s[ws_][:, 8:16, :], w3[ex].rearrange("(kc p) n -> p kc n", p=128)[:, 8:16, :])], W3s[ws_], writes=[W3s[ws_]])
                k.dma("pool", [(W2s[ws_][:, 0:2, :], w2[ex].rearrange("(hc p) n -> p hc n", p=128)[:, 0:2, :]), (W2s[ws_][:, 2:4, :], w2[ex].rearrange("(hc p) n -> p hc n", p=128)[:, 2:4, :])], W2s[ws_], writes=[W2s[ws_]])
                for j in range(4):
                    sl_ = nb_ % 2
                    nb_ += 1
                    xi_, xt_ = xin[sl_], xT[sl_]
                    k.dma_fn("pool", [lambda e, xi_=xi_, ex=ex, j=j: e.indirect_dma_start(out=xi_[:, :], out_offset=None, in_=H_d[:, :],
                                                                                        in_offset=bass.IndirectOffsetOnAxis(ap=idxin[:, ex, j:j + 1], axis=0))],
                             xi_, reads=[H_d, idxin], writes=[xi_])
                    for half in range(2):
                        for c in range(8):
                            kc = half * 8 + c
                            k.op("pe", lambda e, half=half, c=c, kc=kc, xi_=xi_: e.transpose(out=pXT[half][:, c, :], in_=xi_[:, kc * 128:(kc + 1) * 128], identity=ident[:]), reads=[xi_, ident], writes=[pXT[half]], inc=(c == 7))
                        cp(k, "act" if half == 0 else "dve", xt_[:, half * 8:(half + 1) * 8, :], pXT[half][:], [pXT[half]], [xt_])
                    for (Wt, ph) in ((W1s[ws_], pH1), (W3s[ws_], pH3)):
                        for hc in range(4):
                            for kc in range(16):
                                k.op("pe", lambda e, ph=ph, hc=hc, kc=kc, Wt=Wt, xt_=xt_: e.matmul(ph[:, hc, :], lhsT=Wt[:, kc, hc * 128:(hc + 1) * 128], rhs=xt_[:, kc, :], start=(kc == 0), stop=(kc == 15)),
                                     reads=[Wt, xt_], writes=[ph], inc=(kc == 15 and hc == 3))
                    k.op("act", lambda e: e.activation(out=slu[:], in_=pH1[:].rearrange("p a b -> p (a b)"), func=AF.Silu), reads=[pH1], writes=[slu])
                    k.op("dve", lambda e: e.tensor_tensor(out=gT[:].rearrange("p a b -> p (a b)"), in0=slu[:], in1=pH3[:].rearrange("p a b -> p (a b)"), op=ALU.mult), reads=[slu, pH3], writes=[gT])
                    yt_ = Yt[sl_]
                    for nb in range(4):
                        for hc in range(4):
                            k.op("pe", lambda e, nb=nb, hc=hc, ws_=ws_: e.matmul(pYo[nb][:], lhsT=gT[:, hc, :], rhs=W2s[ws_][:, hc, nb * 512:(nb + 1) * 512], start=(hc == 0), stop=(hc == 3)),
                                 reads=[gT, W2s[ws_]], writes=[pYo[nb]], inc=(hc == 3))
                        cp(k, "act" if nb % 2 == 0 else "dve", yt_[nb // 2][:, (nb % 2) * 512:(nb % 2 + 1) * 512], pYo[nb][:], [pYo[nb]], [yt_[nb // 2]])
                    for hh, Yd_ in enumerate((Y_dA, Y_dB)):
                        k.dma_fn("pool", [lambda e, yt_=yt_, ex=ex, j=j, Yd_=Yd_, hh=hh: e.indirect_dma_start(out=Yd_[:, :], out_offset=bass.IndirectOffsetOnAxis(ap=idxout[:, ex, j:j + 1], axis=0),
                                                                                        in_=yt_[hh][:, :], in_offset=None, bounds_check=bcreg(e), oob_is_err=False)],
                                 yt_[hh], reads=[yt_[hh], idxout], writes=[Yd_])

        if upto >= 7:
          out_own = outp("y_out", [NOWN, D], F32)
          with k.phase():
            wpg = k.sb("wpg", [128, 16, D], BF16)
            wpg_v = w_ple_gate.rearrange("(kc p) n -> p kc n", p=128)
            k.dma("pool", [(wpg[:, 4 * q:4 * q + 4, :], wpg_v[:, 4 * q:4 * q + 4, :]) for q in range(4)], wpg, writes=[wpg])
            wpp = k.sb("wpp", [128, 2, D], BF16)
            k.dma("pool", [(wpp[:], w_ple_proj.rearrange("(c p) n -> p c n", p=128))], wpp, writes=[wpp])
            gpl = k.sb("gpl", [128, D], F32); gfin = k.sb("gfin", [128, D], F32)
            k.dma("sp", [(gpl[:], g_ple.partition_broadcast(128))], gpl, writes=[gpl])
            k.dma("sp", [(gfin[:], g_final.partition_broadcast(128))], gfin, writes=[gfin])
            x1s = [k.sb("x1s%d" % i, [128, D], F32) for i in range(2)]
            y1s = [k.sb("y1s%d" % i, [128, D], F32) for i in range(2)]
            y2s = [k.sb("y2s%d" % i, [128, D], F32) for i in range(2)]
            x3 = k.sb("x3", [128, D], F32); ot = [k.sb("ot%d" % i, [128, D], F32) for i in range(2)]
            h3b = k.sb("h3b", [128, D], BF16); h3T = k.sb("h3T", [128, 16, 128], BF16)
            pt = [k.sb("pt%d" % i, [128, 256], F32) for i in range(2)]; ptb = k.sb("ptb", [128, 256], BF16); pTT = k.sb("pTT", [128, 2, 128], BF16)
            sgt = k.sb("sgt", [128, 512], F32); tg = k.sb("tg", [128, 512], F32)
            jk7 = k.sb("jk7", [128, D], BF16); ms7 = k.sb("ms7", [128, 1], F32)
            pT7 = [k.ps("pT7%d" % i, [128, 8, 128], BF16) for i in range(2)]
            pGa = [k.ps("pGa%d" % i, [128, 512], F32) for i in range(2)]
            pPp = [k.ps("pPp%d" % i, [128, 512], F32) for i in range(2)]
            def rms(xb, x_ap, gb, out_b, out_ap):
                k.op("act", lambda e: e.activation(out=jk7[:], in_=x_ap, func=AF.Square, accum_out=ms7[:]), reads=[xb], writes=[jk7, ms7])
                k.op("dve", lambda e: e.tensor_scalar(out=ms7[:], in0=ms7[:], scalar1=1.0 / D, scalar2=EPS, op0=ALU.mult, op1=ALU.add), reads=[ms7], writes=[ms7])
                k.op("act", lambda e: e.activation(out=ms7[:], in_=ms7[:], func=AF.Sqrt), reads=[ms7], writes=[ms7])
                k.op("dve", lambda e: e.reciprocal(out=ms7[:], in_=ms7[:]), reads=[ms7], writes=[ms7])
                k.op("dve", lambda e: e.scalar_tensor_tensor(out=out_ap, in0=x_ap, scalar=ms7[:], in1=gb[:], op0=ALU.mult, op1=ALU.mult), reads=[xb, ms7, gb], writes=[out_b])
            for it in range(16):
                s_ = it % 2
                xa, ya, yb, pa, oa = x1s[s_], y1s[s_], y2s[s_], pt[s_], ot[s_]
                k.dma("sp", [(xa[:], X1_d[it * 128:(it + 1) * 128, :])], xa, reads=[X1_d], writes=[xa])
                k.dma("sp", [(pa[:], po[it * 128:(it + 1) * 128, :])], pa, writes=[pa])
                for (yy, kk) in ((ya, 0), (yb, 1)):
                    if "nogather" in dbg:
                        continue
                    k.dma_fn("pool", [lambda e, yy=yy, kk=kk, it=it, Yd_=Yd_, hh=hh: e.indirect_dma_start(out=yy[:, hh * 1024:(hh + 1) * 1024], out_offset=None, in_=Yd_[:, :],
                                                                                        in_offset=bass.IndirectOffsetOnAxis(ap=dest_i[:, it, kk:kk + 1], axis=0))
                                      for hh, Yd_ in enumerate((Y_dA, Y_dB))],
                             yy, reads=[Y_dA, Y_dB, dest_i, yy], writes=[yy])
                k.op("dve", lambda e, xa=xa, ya=ya, it=it: e.scalar_tensor_tensor(out=xa[:], in0=ya[:], scalar=wts[:, it, 0:1], in1=xa[:], op0=ALU.mult, op1=ALU.add), reads=[ya, wts, xa], writes=[xa])
                k.op("dve", lambda e, xa=xa, yb=yb, it=it: e.scalar_tensor_tensor(out=xa[:], in0=yb[:], scalar=wts[:, it, 1:2], in1=xa[:], op0=ALU.mult, op1=ALU.add), reads=[yb, wts, xa], writes=[xa])
                if "st1" in dbg:
                    k.dma("act", [(out_own[it * 128:(it + 1) * 128, :], xa[:])], xa, reads=[xa])
                    continue
                rms(xa, xa[:], gpl, h3b, h3b[:])
                if "st2" in dbg:
                    k.dma("act", [(out_own[it * 128:(it + 1) * 128, :], xa[:])], xa, reads=[xa])
                    continue
                for half in range(2):
                    for c in range(8):
                        kc = half * 8 + c
                        k.op("pe", lambda e, half=half, c=c, kc=kc: e.transpose(out=pT7[half][:, c, :], in_=h3b[:, kc * 128:(kc + 1) * 128], identity=ident[:]), reads=[h3b, ident], writes=[pT7[half]], inc=(c == 7))
                    cp(k, "act" if half == 0 else "dve", h3T[:, half * 8:(half + 1) * 8, :], pT7[half][:], [pT7[half]], [h3T])
                cp(k, "pool", ptb[:], pa[:], [pa], [ptb])
                for c in range(2):
                    k.op("pe", lambda e, c=c: e.transpose(out=pT7[0][:, c, :], in_=ptb[:, c * 128:(c + 1) * 128], identity=ident[:]), reads=[ptb, ident], writes=[pT7[0]], inc=(c == 1))
                cp(k, "act", pTT[:], pT7[0][:, 0:2, :], [pT7[0]], [pTT])
                for nb in range(4):
                    pg_, pq_ = pGa[nb % 2], pPp[nb % 2]
                    cs = slice(nb * 512, (nb + 1) * 512)
                    for kc in range(16):
                        k.op("pe", lambda e, pg_=pg_, kc=kc, cs=cs: e.matmul(pg_[:], lhsT=h3T[:, kc, :], rhs=wpg[:, kc, cs], start=(kc == 0), stop=(kc == 15)), reads=[h3T, wpg], writes=[pg_], inc=(kc == 15))
                    for c in range(2):
                        k.op("pe", lambda e, pq_=pq_, c=c, cs=cs: e.matmul(pq_[:], lhsT=pTT[:, c, :], rhs=wpp[:, c, cs], start=(c == 0), stop=(c == 1)), reads=[pTT, wpp], writes=[pq_], inc=(c == 1))
                    k.op("act", lambda e, pg_=pg_: e.activation(out=sgt[:], in_=pg_[:], func=AF.Sigmoid), reads=[pg_], writes=[sgt])
                    k.op("dve", lambda e, pq_=pq_: e.tensor_tensor(out=tg[:], in0=sgt[:], in1=pq_[:], op=ALU.mult), reads=[sgt, pq_], writes=[tg])
                    k.op("pool", lambda e, cs=cs, xa=xa: e.tensor_tensor(out=x3[:, cs], in0=tg[:], in1=xa[:, cs], op=ALU.add), reads=[tg, xa], writes=[x3])
                rms(x3, x3[:], gfin, oa, oa[:])
                k.dma("act", [(out_own[it * 128:(it + 1) * 128, :], oa[:])], oa, reads=[oa])

        if "z" in dbg:
            for nm, t in (("QT", QT_d), ("KT", KT_d)):
              with k.phase():
                o = outp("dbg_" + nm, [NH, 128, S], BF16)
                tmp = k.sb("dbgt" + nm, [128, NH, S], BF16)
                k.dma("sp", [(tmp[:], t[:].rearrange("h p n -> p h n"))], tmp, reads=[t], writes=[tmp])
                k.dma("sp", [(o.rearrange("h p n -> p h n"), tmp[:])], tmp, reads=[tmp])
            with k.phase():
                o = outp("dbg_V", [S, AW], BF16)
                tmp = k.sb("dbgtV", [128, NT, AW], BF16)
                k.dma("sp", [(tmp[:], V_d[:].rearrange("(t p) n -> p t n", p=128))], tmp, reads=[V_d], writes=[tmp])
                k.dma("sp", [(o.rearrange("(t p) n -> p t n", p=128), tmp[:])], tmp, reads=[tmp])
            with k.phase():
                o = outp("dbg_U", [128, NG * 512], BF16)
                tmp = k.sb("dbgtU", [128, NG * 512], BF16)
                k.dma("sp", [(tmp[:], U_d[:].rearrange("s h g c -> (s h) (g c)"))], tmp, reads=[U_d], writes=[tmp])
                k.dma("sp", [(o, tmp[:])], tmp, reads=[tmp])

        k.finish(final)
    return nc, I, O


_NC_CACHE = {}


def _core_inputs(inp, c):
    b, hf = c // 2, c % 2
    perm = own_token_perm(hf)
    m = {}
    xb = np.ascontiguousarray(inp["x"][b], dtype=np.float32)
    m["xg"] = xb
    m["xo"] = np.ascontiguousarray(xb[perm])
    m["po"] = np.ascontiguousarray(np.asarray(inp["p"][0, b], dtype=np.float32)[perm])
    cc = np.zeros((128, 4), np.float32)
    cc[:, 0] = 1 - hf
    cc[:, 1] = hf
    m["cc"] = cc
    m["ohlag"] = _lag_onehot(hf)
    f = lambda a: np.ascontiguousarray(np.asarray(a, dtype=np.float32))
    m["rel_bias"] = f(inp["rel_bias"])
    m["lamv"] = f(np.stack([inp["lam_q1"][0], inp["lam_k1"][0], inp["lam_q2"][0], inp["lam_k2"][0]]))
    for nm in ("g_mix", "w_in", "subln_g", "ssm_lam_re", "ssm_lam_im", "ssm_log_dt", "ssm_b_re", "ssm_b_im", "ssm_c_re", "ssm_c_im",
               "ssm_d", "w_glu", "b_glu", "ssm_norm_g", "w_o", "g_ffn", "w1", "w3", "w2", "g_ple", "w_ple_gate", "w_ple_proj"):
        m[nm] = f(inp[nm][0])
    m["g_final"] = f(inp["g_final"])
    m["wr"] = f(np.concatenate([inp["w_router_g"][0], inp["w_router_e"][0]], axis=1))
    m["br"] = f(np.concatenate([inp["b_router_g"][0], inp["b_router_e"][0]]))
    return m


def kernel(**inputs):
    if "nc" not in _NC_CACHE:
        _NC_CACHE["nc"] = build_nc()
    nc, I, O = _NC_CACHE["nc"]
    in_maps = []
    for c in range(8):
        m = _core_inputs(inputs, c)
        in_maps.append({k_: v for k_, v in m.items() if k_ in I})
    res = run_bass_kernel_spmd(nc, in_maps, core_ids=list(range(8)))
    out = np.zeros((4, S, D), np.float32)
    for c in range(8):
        b, hf = c // 2, c % 2
        out[b][own_token_perm(hf)] = np.asarray(res.results[c]["y_out"], dtype=np.float32)
    return out
```

```python
import contextlib
import math
import numpy as np
import concourse.bass as bass
import concourse.mybir as mybir
from concourse.bass_utils import run_bass_kernel_spmd

F32 = mybir.dt.float32
BF16 = mybir.dt.bfloat16
I32 = mybir.dt.int32
U32 = mybir.dt.uint32
AF = mybir.ActivationFunctionType
ALU = mybir.AluOpType
AX = mybir.AxisListType


class Buf:
    __slots__ = ("name", "t", "writers", "readers", "dsem")

    def __init__(self, name, t=None):
        self.name = name
        self.t = t
        self.writers = {}
        self.readers = {}
        self.dsem = None

    def __getitem__(self, idx):
        return self.t[idx]


class K:
    ENGS = ("pe", "act", "dve", "pool", "sp")

    def __init__(self, nc, stack):
        self.nc = nc
        self.stack = stack
        self.main_stack = stack
        self.free_dsems = []
        self.phase_bufs = None
        self.ops = {e: [] for e in self.ENGS}
        self.sems = {}
        self.count = {}
        self.seen = {e: {} for e in self.ENGS}
        self.pending = {e: False for e in self.ENGS}
        for e in self.ENGS:
            self._mksem("E_" + e)

    def _mksem(self, key):
        if key.startswith("D") and self.free_dsems:
            return self.free_dsems.pop()
        s = self.main_stack.enter_context(self.nc.semaphore("s_" + key))
        self.sems[key] = s
        self.count[key] = 0
        return key

    def sb(self, name, shape, dtype):
        self._n = getattr(self, "_n", 0) + 1
        name = "%s_%d" % (name, self._n)
        t = self.stack.enter_context(self.nc.sbuf_tensor(name, list(shape), dtype))
        b = Buf(name, t)
        if self.phase_bufs is not None:
            self.phase_bufs.append(b)
        return b

    def ps(self, name, shape, dtype=F32):
        self._n = getattr(self, "_n", 0) + 1
        name = "%s_%d" % (name, self._n)
        t = self.stack.enter_context(self.nc.psum_tensor(name, list(shape), dtype))
        b = Buf(name, t)
        if self.phase_bufs is not None:
            self.phase_bufs.append(b)
        return b

    def dram(self, name, shape, dtype, kind="Internal"):
        t = self.nc.dram_tensor(name, list(shape), dtype, kind=kind)
        return Buf(name, t.ap())

    def _waits(self, eng, reads, writes):
        need = {}
        for b in reads:
            for k, v in b.writers.items():
                if need.get(k, 0) < v:
                    need[k] = v
        for b in writes:
            for k, v in b.writers.items():
                if need.get(k, 0) < v:
                    need[k] = v
            for k, v in b.readers.items():
                if need.get(k, 0) < v:
                    need[k] = v
        out = []
        seen = self.seen[eng]
        own = "E_" + eng
        for k, v in need.items():
            if eng == "pe" and k == own:
                continue
            if seen.get(k, 0) < v:
                seen[k] = v
                out.append((k, v))
        return out

    def op(self, eng, fn, reads=(), writes=(), inc=True):
        waits = self._waits(eng, reads, writes)
        key = "E_" + eng
        val = self.count[key] + 1
        if inc:
            self.count[key] = val
            self.pending[eng] = False
        else:
            self.pending[eng] = True
        self.ops[eng].append((waits, fn, key if inc else None, 1))
        for b in writes:
            b.writers = {key: val}
            b.readers = {}
        for b in reads:
            if b.readers.get(key, 0) < val:
                b.readers[key] = val

    def dma(self, q, parts, sbuf, reads=(), writes=()):
        waits = self._waits(q, reads, writes)
        if sbuf.dsem is None:
            sbuf.dsem = self._mksem("D%d_%s" % (len(self.sems), sbuf.name))
        key = sbuf.dsem
        first = True
        for p in parts:
            o, i = p[0], p[1]
            kw = p[2] if len(p) > 2 else {}
            self.count[key] += 16
            fn = (lambda e, o=o, i=i, kw=kw: e.dma_start(out=o, in_=i, **kw))
            self.ops[q].append((waits if first else [], fn, key, 16))
            first = False
        val = self.count[key]
        for b in writes:
            b.writers = {key: val}
            b.readers = {}
        for b in reads:
            if b.readers.get(key, 0) < val:
                b.readers[key] = val

    def dma_fn(self, q, fns, sbuf, reads=(), writes=()):
        waits = self._waits(q, reads, writes)
        if sbuf.dsem is None:
            sbuf.dsem = self._mksem("D%d_%s" % (len(self.sems), sbuf.name))
        key = sbuf.dsem
        first = True
        for fn in fns:
            self.count[key] += 16
            self.ops[q].append((waits if first else [], fn, key, 16))
            first = False
        val = self.count[key]
        for b in writes:
            b.writers = {key: val}
            b.readers = {}
        for b in reads:
            if b.readers.get(key, 0) < val:
                b.readers[key] = val

    def barrier(self):
        for e in self.ENGS:
            assert not self.pending[e], e
        for e in self.ENGS:
            waits = []
            seen = self.seen[e]
            for key, cnt in self.count.items():
                if cnt > 0 and seen.get(key, 0) < cnt and not (e == "pe" and key == "E_pe"):
                    seen[key] = cnt
                    waits.append((key, cnt))
            if waits:
                self.ops[e].append((waits, None, None, 0))

    @contextlib.contextmanager
    def phase(self):
        outer = self.stack
        outer_bufs = self.phase_bufs
        with contextlib.ExitStack() as ph:
            self.stack = ph
            self.phase_bufs = []
            yield
            self.barrier()
            for b in self.phase_bufs:
                if b.dsem is not None:
                    self.free_dsems.append(b.dsem)
                    b.dsem = None
            self.phase_bufs = outer_bufs
            self.stack = outer
            self.flush()

    def flush(self):
        for e in self.ENGS:
            assert not self.pending[e], "engine %s ends with non-incrementing op" % e

        def run(engname):
            lst = self.ops[engname]

            def body(eng):
                for waits, fn, key, n in lst:
                    for k, v in waits:
                        eng.wait_ge(self.sems[k], v)
                    if fn is None:
                        continue
                    ins = fn(eng)
                    if key is not None:
                        ins.then_inc(self.sems[key], n)
            return body

        with self.nc.Block() as block:
            block.sync(run("sp"))
            block.scalar(run("act"))
            block.vector(run("dve"))
            block.gpsimd(run("pool"))
            block.tensor(run("pe"))
        self.ops = {e: [] for e in self.ENGS}

    def finish(self, final_waits):
        for b in final_waits:
            w = self._waits("sp", [b], [b])
            self.ops["sp"].append((w, None, None, 0))
        self.barrier()
        self.flush()


def cp(k, eng, out, in_, reads, writes, inc=True):
    if eng == "act":
        k.op("act", lambda e: e.copy(out=out, in_=in_), reads, writes, inc)
    else:
        k.op(eng, lambda e: e.tensor_copy(out=out, in_=in_), reads, writes, inc)


D = 2048
S = 4096
NT = 32
NOWN = 2048
AW = 1024
NH = 8
DK = 64
DV = 128
NG = 64
NE = 32
DE = 512
CAP = 256
LAM_INIT = 0.8 - 0.6 * math.exp(-0.3 * 0)
EPS = 1e-6


def _t5_bucket(n):
    n = np.maximum(n, 0)
    nf = np.maximum(n, 1).astype(np.float32)
    large = 16 + (np.log(nf / np.float32(16)) / np.float32(math.log(128 / 16)) * np.float32(16)).astype(np.int32)
    large = np.minimum(large, 31)
    return np.where(n < 16, n, large)


def _lag_onehot(hf):
    oh = np.zeros((33, 768), np.float32)
    for m in range(3):
        delta = hf + 1 - m
        for n1 in range(256):
            lag = delta * 128 + n1 - 128
            c = m * 256 + n1
            if lag < 0:
                oh[32, c] = 1.0
            else:
                oh[int(_t5_bucket(np.array([lag]))[0]), c] += 1.0
                oh[31, c] -= 1.0
    return oh


def own_token_perm(hf):
    n = np.arange(NOWN)
    glob = 128 * (2 * (n // 128) + hf) + (n % 128)
    cb, l = n // 1024, n % 1024
    pos = cb * 1024 + (l % 8) * 128 + (l // 8)
    perm = np.zeros(NOWN, np.int64)
    perm[pos] = glob
    return perm


def build_nc(upto=99, dbg=()):
    nc = bass.Bass("TRN2", target_bir_lowering=False)
    I = {}

    def inp(name, shape, dt=F32):
        I[name] = nc.dram_tensor(name, list(shape), dt, kind="ExternalInput").ap()
        return I[name]

    xg = inp("xg", [S, D])
    xo = inp("xo", [NOWN, D])
    po = inp("po", [NOWN, 256])
    cc = inp("cc", [128, 4])
    ohlag = inp("ohlag", [33, 768])
    rel_bias = inp("rel_bias", [32, 8])
    g_mix = inp("g_mix", [D])
    w_in = inp("w_in", [D, 4096])
    lamv = inp("lamv", [4, 64])
    subln_g = inp("subln_g", [128])
    ssm_lam_re = inp("ssm_lam_re", [64, 64])
    ssm_lam_im = inp("ssm_lam_im", [64, 64])
    ssm_log_dt = inp("ssm_log_dt", [64])
    ssm_b_re = inp("ssm_b_re", [64, 64, 16])
    ssm_b_im = inp("ssm_b_im", [64, 64, 16])
    ssm_c_re = inp("ssm_c_re", [64, 16, 64])
    ssm_c_im = inp("ssm_c_im", [64, 16, 64])
    ssm_d = inp("ssm_d", [64, 16])
    w_glu = inp("w_glu", [1024, 1024])
    b_glu = inp("b_glu", [1024])
    ssm_norm_g = inp("ssm_norm_g", [1024])
    w_o = inp("w_o", [D, D])
    g_ffn = inp("g_ffn", [D])
    wr = inp("wr", [D, 36])
    br = inp("br", [36])
    w1 = inp("w1", [NE, D, DE])
    w3 = inp("w3", [NE, D, DE])
    w2 = inp("w2", [NE, DE, D])
    g_ple = inp("g_ple", [D])
    w_ple_gate = inp("w_ple_gate", [D, D])
    w_ple_proj = inp("w_ple_proj", [256, D])
    g_final = inp("g_final", [D])
    O = {}

    def outp(name, shape, dt=F32):
        O[name] = nc.dram_tensor(name, list(shape), dt, kind="ExternalOutput").ap()
        return O[name]

    with contextlib.ExitStack() as st:
        k = K(nc, st)
        hT_d = k.dram("hT_d", [NT, 128, 16, 128], BF16)
        QT_d = k.dram("QT_d", [NH, 128, S], BF16)
        KT_d = k.dram("KT_d", [NH, 128, S], BF16)
        V_d = k.dram("V_d", [S, AW], BF16)
        U_d = k.dram("U_d", [8, 16, NG, 512], BF16)
        E_d = k.dram("E_d", [NH, 3, 129 * 256], F32)
        catT_d = k.dram("catT_d", [16, 128, NOWN], BF16)
        X1_d = k.dram("X1_d", [NOWN, D], F32)
        NROW = NE * CAP
        H_d = k.dram("H_d", [NROW, D], BF16)
        Y_dA = k.dram("Y_dA", [NROW, D // 2], F32)
        Y_dB = k.dram("Y_dB", [NROW, D // 2], F32)
        dest_i = k.sb("dest_i", [128, 16, 2], I32)
        wts = k.sb("wts", [128, 16, 2], F32)
        idxin = k.sb("idxin", [128, NE, 4], I32)
        idxout = k.sb("idxout", [128, NE, 4], I32)
        H2_d = k.dram("H2_d", [NOWN, D], BF16)
        ident = k.sb("ident", [128, 128], BF16)
        identf = k.sb("identf", [128, 128], F32)
        ccs = k.sb("ccs", [128, 4], F32)
        lam_t = k.sb("lam_t", [128, 1], F32)
        E_sb = k.sb("E_sb", [128, NH, 3, 128], F32)
        final = []
        _bc = {}

        def bcreg(e):
            if "r" not in _bc:
                _bc["r"] = e.to_reg(NE * CAP - 1)
            return _bc["r"]

        with k.phase():
            k.op("pool", lambda e: e.memset(identf[:], 0.0), writes=[identf])
            k.op("pool", lambda e: e.affine_select(out=identf[:], in_=identf[:], pattern=[[-1, 128]],
                                                   compare_op=ALU.not_equal, fill=1.0, base=0,
                                                   channel_multiplier=1), reads=[identf], writes=[identf])
            cp(k, "dve", ident[:], identf[:], [identf], [ident])
            k.dma("sp", [(ccs[:], cc)], ccs, writes=[ccs])
            lv = k.sb("lv", [128, 4, 64], F32)
            k.dma("sp", [(lv[:], lamv.rearrange("a b -> (a b)").partition_broadcast(128).rearrange("p (a b) -> p a b", a=4))], lv, writes=[lv])
            lj = k.sb("lj", [128, 64], F32)
            l2 = k.sb("l2", [128, 2], F32)
            for i in range(2):
                k.op("dve", lambda e, i=i: e.tensor_tensor(out=lj[:], in0=lv[:, 2 * i, :], in1=lv[:, 2 * i + 1, :], op=ALU.mult), reads=[lv], writes=[lj])
                k.op("dve", lambda e, i=i: e.tensor_reduce(out=l2[:, i:i + 1], in_=lj[:], axis=AX.X, op=ALU.add), reads=[lj], writes=[l2])
            k.op("act", lambda e: e.activation(out=l2[:], in_=l2[:], func=AF.Exp), reads=[l2], writes=[l2])
            k.op("dve", lambda e: e.tensor_tensor(out=lam_t[:], in0=l2[:, 0:1], in1=l2[:, 1:2], op=ALU.subtract), reads=[l2], writes=[lam_t])
            k.op("dve", lambda e: e.tensor_scalar(out=lam_t[:], in0=lam_t[:], scalar1=LAM_INIT, scalar2=None, op0=ALU.add), reads=[lam_t], writes=[lam_t])
            tb = k.sb("tb", [64, 8], F32)
            k.op("pool", lambda e: e.memset(tb[:], -30000.0), writes=[tb])
            k.dma("sp", [(tb[0:32, :], rel_bias)], tb, reads=[tb], writes=[tb])
            ohs = k.sb("ohs", [64, 768], F32)
            k.op("pool", lambda e: e.memset(ohs[:], 0.0), writes=[ohs])
            k.dma("sp", [(ohs[0:33, :], ohlag)], ohs, reads=[ohs], writes=[ohs])
            pe_ = k.ps("pe_", [8, 768], F32)
            k.op("pe", lambda e: e.matmul(pe_[:, 0:384], lhsT=tb[:], rhs=ohs[:, 0:384], start=True, stop=True), reads=[tb, ohs], writes=[pe_], inc=False)
            k.op("pe", lambda e: e.matmul(pe_[:, 384:768], lhsT=tb[:], rhs=ohs[:, 384:768], start=True, stop=True), reads=[tb, ohs], writes=[pe_])
            es = k.sb("es", [8, 768], F32)
            k.op("act", lambda e: e.activation(out=es[:], in_=pe_[:], func=AF.Exp), reads=[pe_], writes=[es])
            parts = []
            for m in range(3):
                parts.append((E_d[:, m, :].rearrange("h (r n) -> h r n", n=256),
                              es[:, m * 256:(m + 1) * 256].unsqueeze(1).to_broadcast([8, 129, 256])))
            k.dma("sp", parts, es, reads=[es], writes=[E_d])
            parts = []
            for h in range(NH):
                for m in range(3):
                    src = E_d[h, m]
                    parts.append((E_sb[:, h, m, :], bass.AP(src.tensor, src.offset + 128, [[255, 128], [1, 128]])))
            k.dma("sp", parts, E_sb, reads=[E_d], writes=[E_sb])
            if "E" in dbg:
                k.dma("sp", [(outp("dbg_E", [128, NH * 3 * 128]), E_sb[:].rearrange("p a b c -> p (a b c)"))], E_sb, reads=[E_sb])
                k.dma("sp", [(outp("dbg_lam", [128, 1]), lam_t[:])], lam_t, reads=[lam_t])
                final += [E_sb, lam_t]

        def sin_red(outb, out_ap, angb, ang_ap, shape, tmps):
            tq, ti, tr, tm = tmps
            sl = tuple(slice(0, n) for n in shape)
            k.op("dve", lambda e: e.tensor_scalar(out=tq[sl], in0=ang_ap, scalar1=1.0 / 6.283185307179586, scalar2=None, op0=ALU.mult), reads=[angb], writes=[tq])
            k.op("dve", lambda e: e.tensor_copy(out=ti[sl], in_=tq[sl]), reads=[tq], writes=[ti])
            k.op("dve", lambda e: e.tensor_copy(out=tq[sl], in_=ti[sl]), reads=[ti], writes=[tq])
            k.op("dve", lambda e: e.scalar_tensor_tensor(out=tr[sl], in0=tq[sl], scalar=-6.283185307179586, in1=ang_ap, op0=ALU.mult, op1=ALU.add), reads=[tq, angb], writes=[tr])
            k.op("dve", lambda e: e.tensor_scalar(out=tm[sl], in0=tr[sl], scalar1=math.pi, scalar2=-6.283185307179586, op0=ALU.is_gt, op1=ALU.mult), reads=[tr], writes=[tm])
            k.op("dve", lambda e: e.tensor_tensor(out=tr[sl], in0=tr[sl], in1=tm[sl], op=ALU.add), reads=[tr, tm], writes=[tr])
            k.op("dve", lambda e: e.tensor_scalar(out=tm[sl], in0=tr[sl], scalar1=-math.pi, scalar2=6.283185307179586, op0=ALU.is_lt, op1=ALU.mult), reads=[tr], writes=[tm])
            k.op("dve", lambda e: e.tensor_tensor(out=tr[sl], in0=tr[sl], in1=tm[sl], op=ALU.add), reads=[tr, tm], writes=[tr])
            k.op("dve", lambda e: e.tensor_scalar(out=tr[sl], in0=tr[sl], scalar1=-3.1415925, scalar2=3.1415925, op0=ALU.max, op1=ALU.min), reads=[tr], writes=[tr])
            k.op("act", lambda e: e.activation(out=out_ap, in_=tr[sl], func=AF.Sin), reads=[tr], writes=[outb])

        if upto >= 1:
          with k.phase():
            gm = k.sb("gm", [128, D], F32)
            k.dma("act", [(gm[:], g_mix.partition_broadcast(128))], gm, writes=[gm])
            xt = [k.sb("xt%d" % i, [128, D], F32) for i in range(2)]
            hb = [k.sb("hb%d" % i, [128, D], BF16) for i in range(2)]
            junk = k.sb("junk", [128, D], BF16)
            ssq = [k.sb("ssq%d" % i, [128, 1], F32) for i in range(2)]
            hTs = [k.sb("hTs%d" % i, [128, 16, 128], BF16) for i in range(2)]
            pT = [k.ps("pT%d" % i, [128, 8, 128], BF16) for i in range(2)]
            for i in range(NT):
                s = i % 2
                k.dma("sp", [(xt[s][:], xg[i * 128:(i + 1) * 128, :])], xt[s], writes=[xt[s]])
                k.op("act", lambda e, s=s: e.activation(out=junk[:], in_=xt[s][:], func=AF.Square, accum_out=ssq[s][:]),
                     reads=[xt[s]], writes=[junk, ssq[s]])
                k.op("dve", lambda e, s=s: e.tensor_scalar(out=ssq[s][:], in0=ssq[s][:], scalar1=1.0 / D, scalar2=EPS, op0=ALU.mult, op1=ALU.add),
                     reads=[ssq[s]], writes=[ssq[s]])
                k.op("act", lambda e, s=s: e.activation(out=ssq[s][:], in_=ssq[s][:], func=AF.Sqrt), reads=[ssq[s]], writes=[ssq[s]])
                k.op("dve", lambda e, s=s: e.reciprocal(out=ssq[s][:], in_=ssq[s][:]), reads=[ssq[s]], writes=[ssq[s]])
                k.op("dve", lambda e, s=s: e.scalar_tensor_tensor(out=hb[s][:], in0=xt[s][:], scalar=ssq[s][:], in1=gm[:], op0=ALU.mult, op1=ALU.mult),
                     reads=[xt[s], ssq[s], gm], writes=[hb[s]])
                for half in range(2):
                    for c in range(8):
                        kc = half * 8 + c
                        k.op("pe", lambda e, s=s, half=half, c=c, kc=kc: e.transpose(out=pT[half][:, c, :], in_=hb[s][:, kc * 128:(kc + 1) * 128], identity=ident[:]),
                             reads=[hb[s], ident], writes=[pT[half]], inc=(c == 7))
                    cp(k, "act" if half == 0 else "dve", hTs[s][:, half * 8:(half + 1) * 8, :], pT[half][:], [pT[half]], [hTs[s]])
                k.dma("act", [(hT_d[i], hTs[s][:])], hTs[s], reads=[hTs[s]], writes=[hT_d])

        if upto >= 2:
          with k.phase():
            wb = [k.sb("wb%d" % i, [128, 16, 512], BF16) for i in range(2)]
            hs = [k.sb("hs%d" % i, [128, 4, 16, 128], BF16) for i in range(2)]
            pz = [k.ps("pz%d" % i, [128, 512], F32) for i in range(4)]
            stg = [k.sb("stg%d" % i, [128, 4, 512], BF16) for i in range(2)]
            usb = k.sb("usb", [128, 4, 8, 512], BF16)
            w_v = w_in.rearrange("(kc p) n -> p kc n", p=128)
            nz = 0
            nh = 0
            for cb in range(8):
                ws = wb[cb % 2]
                k.dma("pool", [(ws[:, 0:8, :], w_v[:, 0:8, cb * 512:(cb + 1) * 512]), (ws[:, 8:16, :], w_v[:, 8:16, cb * 512:(cb + 1) * 512])], ws, writes=[ws])
                kind = "QKVU"[cb // 2]
                for stile in range(8):
                    hss = hs[nh % 2]
                    nh += 1
                    k.dma("sp", [(hss[:], hT_d[stile * 4:(stile + 1) * 4].rearrange("t p c n -> p t c n"))], hss, reads=[hT_d], writes=[hss])
                    sg = stg[stile % 2]
                    for m in range(4):
                        pp = pz[nz % 4]
                        nz += 1
                        ev = "act" if nz % 2 == 0 else "dve"
                        if kind == "V":
                            for kc in range(16):
                                k.op("pe", lambda e, pp=pp, hss=hss, ws=ws, m=m, kc=kc: e.matmul(pp[:], lhsT=hss[:, m, kc, :], rhs=ws[:, kc, :], start=(kc == 0), stop=(kc == 15)),
                                     reads=[hss, ws], writes=[pp], inc=(kc == 15))
                            cp(k, ev, sg[:, m, :], pp[:], [pp], [sg])
                        else:
                            for kc in range(16):
                                k.op("pe", lambda e, pp=pp, hss=hss, ws=ws, m=m, kc=kc: e.matmul(pp[:].rearrange("p (t n) -> p t n", t=4), lhsT=ws[:, kc, m * 128:(m + 1) * 128], rhs=hss[:, :, kc, :], start=(kc == 0), stop=(kc == 15)),
                                     reads=[hss, ws], writes=[pp], inc=(kc == 15))
                            if kind == "Q":
                                if ev == "act":
                                    k.op("act", lambda e, sg=sg, pp=pp, m=m: e.activation(out=sg[:, m, :], in_=pp[:], func=AF.Copy, scale=DK ** -0.5), reads=[pp], writes=[sg])
                                else:
                                    k.op("dve", lambda e, sg=sg, pp=pp, m=m: e.tensor_scalar(out=sg[:, m, :], in0=pp[:], scalar1=DK ** -0.5, scalar2=None, op0=ALU.mult), reads=[pp], writes=[sg])
                            elif kind == "K":
                                cp(k, ev, sg[:, m, :], pp[:], [pp], [sg])
                            else:
                                cp(k, ev, usb[:, m, :, stile * 64:(stile + 1) * 64], pp[:].rearrange("p (c s) -> p s c", s=8), [pp], [usb])
                    t0 = stile * 512
                    if kind == "Q" or kind == "K":
                        dst = QT_d if kind == "Q" else KT_d
                        h0 = (cb % 2) * 4
                        k.dma("act", [(dst[h0:h0 + 4, :, t0:t0 + 512].rearrange("h p n -> p h n"), sg[:])], sg, reads=[sg], writes=[dst])
                    elif kind == "V":
                        c0 = (cb % 2) * 512
                        k.dma("act", [(V_d[t0:t0 + 512, c0:c0 + 512].rearrange("(t p) n -> p t n", p=128), sg[:])], sg, reads=[sg], writes=[V_d])
                if kind == "U":
                    g0 = (cb % 2) * 32
                    parts = []
                    for m in range(4):
                        for gl in range(8):
                            parts.append((U_d[:, :, g0 + 8 * m + gl, :].rearrange("s h c -> h s c"), usb[gl * 16:(gl + 1) * 16, m, :, :]))
                    k.dma("act", parts, usb, reads=[usb], writes=[U_d])
        if upto >= 3:
          with k.phase():
            gsub = k.sb("gsub", [128, 128], F32)
            k.dma("sp", [(gsub[:], subln_g.partition_broadcast(128))], gsub, writes=[gsub])
            k.op("dve", lambda e: e.tensor_scalar(out=gsub[:], in0=gsub[:], scalar1=1.0 - LAM_INIT, scalar2=None, op0=ALU.mult), reads=[gsub], writes=[gsub])
            KTs = [k.sb("KTs%d" % i, [128, S], BF16) for i in range(2)]
            QTa = [k.sb("QTa%d" % i, [128, NT, 128], BF16) for i in range(2)]
            QTo = [k.sb("QTo%d" % i, [128, 16, 128], BF16) for i in range(2)]
            Vh = [k.sb("Vh%d" % i, [128, NT, 132], BF16) for i in range(2)]
            aTh = [k.sb("aTh%d" % i, [128, NOWN], BF16) for i in range(2)]
            for i in range(2):
                k.op("pool", lambda e, i=i: e.memset(Vh[i][:], 1.0), writes=[Vh[i]])
            S1 = [k.ps("S1_%d" % i, [128, 4, 128], F32) for i in range(2)]
            S2 = [k.ps("S2_%d" % i, [128, 4, 128], F32) for i in range(2)]
            O1 = k.ps("O1", [128, 512], F32)
            O2 = k.ps("O2", [128, 512], F32)
            pA = k.ps("pA", [128, 128], BF16)
            P1 = [k.sb("P1_%d" % i, [128, 4, 128], BF16) for i in range(2)]
            P2 = [k.sb("P2_%d" % i, [128, 4, 128], BF16) for i in range(2)]
            Pf1 = k.sb("Pf1", [128, 3, 128], F32)
            Pf2 = k.sb("Pf2", [128, 3, 128], F32)
            rr = k.sb("rr", [128, 2], F32)
            tmpo = k.sb("tmpo", [128, 128], F32)
            oo = k.sb("oo", [128, 128], F32)
            jk = k.sb("jk", [128, 128], F32)
            ms = k.sb("ms", [128, 1], F32)
            ab = k.sb("ab", [128, 128], BF16)
            ng = 0
            for h in range(NH):
                hs_ = h % 2
                k.dma("sp", [(KTs[hs_][:], KT_d[h])], KTs[hs_], reads=[KT_d], writes=[KTs[hs_]])
                k.dma("sp", [(QTa[hs_][:].rearrange("p t n -> p (t n)"), QT_d[h])], QTa[hs_], reads=[QT_d], writes=[QTa[hs_]])
                k.dma("act", [(Vh[hs_][:, :, 0:128], V_d[:, h * 128:(h + 1) * 128].rearrange("(t p) n -> p t n", p=128))], Vh[hs_], reads=[V_d], writes=[Vh[hs_]])
                qv = QTa[hs_][:].rearrange("p (j r) n -> p j r n", r=2)
                k.op("pool", lambda e, hs_=hs_, qv=qv: e.tensor_scalar(out=QTo[hs_][:], in0=qv[:, :, 0, :], scalar1=ccs[:, 0:1], scalar2=None, op0=ALU.mult),
                     reads=[QTa[hs_], ccs], writes=[QTo[hs_]])
                k.op("dve", lambda e, hs_=hs_, qv=qv: e.scalar_tensor_tensor(out=QTo[hs_][:], in0=qv[:, :, 1, :], scalar=ccs[:, 1:2], in1=QTo[hs_][:], op0=ALU.mult, op1=ALU.add),
                     reads=[QTa[hs_], ccs, QTo[hs_]], writes=[QTo[hs_]])
                for j in range(16):
                    nplain = max(0, 2 * j - 1)
                    groups = [list(range(a, min(a + 4, nplain))) for a in range(0, nplain, 4)]
                    spec = [kb for kb in (2 * j - 1, 2 * j, 2 * j + 1) if kb >= 0]
                    groups.append(spec)
                    nkb = 2 * j + 2
                    for gi, grp in enumerate(groups):
                        is_spec = (gi == len(groups) - 1)
                        s_ = ng % 2
                        ng += 1
                        for mp, (SS, base) in enumerate(((S1[s_], 0), (S2[s_], 64))):
                            for i, kb in enumerate(grp):
                                k.op("pe", lambda e, SS=SS, base=base, i=i, kb=kb, hs_=hs_, j=j: e.matmul(SS[:, i, :], lhsT=KTs[hs_][base:base + 64, kb * 128:(kb + 1) * 128], rhs=QTo[hs_][base:base + 64, j, :], start=True, stop=True),
                                     reads=[KTs[hs_], QTo[hs_]], writes=[SS], inc=(i == len(grp) - 1))
                        n = len(grp)
                        for mp, (SS, PP, Pf) in enumerate(((S1[s_], P1[s_], Pf1), (S2[s_], P2[s_], Pf2))):
                            if not is_spec:
                                k.op("act", lambda e, SS=SS, PP=PP, n=n: e.activation(out=PP[:, 0:n, :], in_=SS[:, 0:n, :], func=AF.Exp), reads=[SS], writes=[PP])
                            else:
                                k.op("act", lambda e, SS=SS, Pf=Pf, n=n: e.activation(out=Pf[:, 0:n, :], in_=SS[:, 0:n, :], func=AF.Exp), reads=[SS], writes=[Pf])
                                m0 = 3 - n
                                k.op("dve", lambda e, PP=PP, Pf=Pf, n=n, m0=m0, h=h: e.tensor_tensor(out=PP[:, 0:n, :], in0=Pf[:, 0:n, :], in1=E_sb[:, h, m0:3, :], op=ALU.mult),
                                     reads=[Pf, E_sb], writes=[PP])
                        for mp, (PP, OO) in enumerate(((P1[s_], O1), (P2[s_], O2))):
                            for i, kb in enumerate(grp):
                                k.op("pe", lambda e, PP=PP, OO=OO, i=i, kb=kb, hs_=hs_, nkb=nkb: e.matmul(OO[:, 0:129], lhsT=PP[:, i, :], rhs=Vh[hs_][:, kb, 0:129], start=(kb == 0), stop=(kb == nkb - 1)),
                                     reads=[PP, Vh[hs_]], writes=[OO], inc=(i == len(grp) - 1))
                    k.op("dve", lambda e: e.reciprocal(out=rr[:, 0:1], in_=O1[:, 128:129]), reads=[O1], writes=[rr])
                    k.op("dve", lambda e: e.reciprocal(out=rr[:, 1:2], in_=O2[:, 128:129]), reads=[O2, rr], writes=[rr])
                    k.op("dve", lambda e: e.tensor_tensor(out=rr[:, 1:2], in0=rr[:, 1:2], in1=lam_t[:], op=ALU.mult), reads=[rr, lam_t], writes=[rr])
                    k.op("dve", lambda e: e.tensor_scalar(out=tmpo[:], in0=O2[:, 0:128], scalar1=rr[:, 1:2], scalar2=None, op0=ALU.mult), reads=[O2, rr], writes=[tmpo])
                    k.op("dve", lambda e: e.scalar_tensor_tensor(out=oo[:], in0=O1[:, 0:128], scalar=rr[:, 0:1], in1=tmpo[:], op0=ALU.mult, op1=ALU.subtract), reads=[O1, rr, tmpo], writes=[oo])
                    k.op("act", lambda e: e.activation(out=jk[:], in_=oo[:], func=AF.Square, accum_out=ms[:]), reads=[oo], writes=[jk, ms])
                    k.op("dve", lambda e: e.tensor_scalar(out=ms[:], in0=ms[:], scalar1=1.0 / DV, scalar2=1e-5, op0=ALU.mult, op1=ALU.add), reads=[ms], writes=[ms])
                    k.op("act", lambda e: e.activation(out=ms[:], in_=ms[:], func=AF.Sqrt), reads=[ms], writes=[ms])
                    k.op("dve", lambda e: e.reciprocal(out=ms[:], in_=ms[:]), reads=[ms], writes=[ms])
                    k.op("dve", lambda e: e.scalar_tensor_tensor(out=ab[:], in0=oo[:], scalar=ms[:], in1=gsub[:], op0=ALU.mult, op1=ALU.mult), reads=[oo, ms, gsub], writes=[ab])
                    k.op("pe", lambda e: e.transpose(out=pA[:], in_=ab[:], identity=ident[:]), reads=[ab, ident], writes=[pA])
                    cp(k, "act", aTh[hs_][:, j * 128:(j + 1) * 128], pA[:], [pA], [aTh[hs_]])
                k.dma("act", [(catT_d[h], aTh[hs_][:])], aTh[hs_], reads=[aTh[hs_]], writes=[catT_d])
        if "a" in dbg:
            with k.phase():
                o = outp("dbg_aT", [8, 128, NOWN], BF16)
                tmp = k.sb("dbgta", [128, 8, NOWN], BF16)
                k.dma("sp", [(tmp[:], catT_d[0:8].rearrange("h p n -> p h n"))], tmp, reads=[catT_d], writes=[tmp])
                k.dma("sp", [(o.rearrange("h p n -> p h n"), tmp[:])], tmp, reads=[tmp])

        if upto >= 4:
          with k.phase():
            y_tm = [k.sb("y_tm%d" % i, [128, 8, 1024], F32) for i in range(2)]
            wscope = contextlib.ExitStack()
            wphase = k.phase()
            wphase.__enter__()
            PmRe = k.sb("PmRe", [128, 32, 128], BF16)
            PmIm = k.sb("PmIm", [128, 32, 128], BF16)
            Mm = k.sb("Mm", [128, 64, 128], BF16)
            QmRe = k.sb("QmRe", [128, 32, 128], BF16)
            QmIm = k.sb("QmIm", [128, 32, 128], BF16)
            th8 = k.sb("th8", [128, 32], F32)
            dec8 = k.sb("dec8", [128, 32], F32)
            iot = k.sb("iot", [128, 512], F32)
            ioti = k.sb("ioti", [128, 512], I32)
            k.op("pool", lambda e: e.iota(ioti[:], pattern=[[1, 512]], base=0, channel_multiplier=0), writes=[ioti])
            cp(k, "dve", iot[:], ioti[:], [ioti], [iot])
            if True:
              with k.phase():
                    def T(name, shape, dt=F32):
                        return k.sb(name, shape, dt)
                    lre = T("lre", [128, 32]); lim = T("lim", [128, 32]); dtt = T("dtt", [128, 32])
                    with nc.allow_non_contiguous_dma(reason="tiny transposed parameter loads"):
                        for gh in range(2):
                            k.dma("sp", [(lre[gh * 64:(gh + 1) * 64, :], ssm_lam_re[gh * 32:(gh + 1) * 32, :].rearrange("g p -> p g"))], lre, reads=[lre], writes=[lre])
                            k.dma("sp", [(lim[gh * 64:(gh + 1) * 64, :], ssm_lam_im[gh * 32:(gh + 1) * 32, :].rearrange("g p -> p g"))], lim, reads=[lim], writes=[lim])
                            k.dma("sp", [(dtt[gh * 64:(gh + 1) * 64, :], ssm_log_dt[gh * 32:(gh + 1) * 32].partition_broadcast(64))], dtt, reads=[dtt], writes=[dtt])
                        k.flush()
                    k.op("act", lambda e: e.activation(out=dtt[:], in_=dtt[:], func=AF.Exp), reads=[dtt], writes=[dtt])
                    tq = T("tq", [128, 32]); ti = T("ti", [128, 32], I32); tr = T("tr", [128, 32]); tm = T("tm", [128, 32])
                    tmps = (tq, ti, tr, tm)
                    mag = T("mag", [128, 32]); th = T("th", [128, 32]); thc = T("thc", [128, 32])
                    s1 = T("s1", [128, 32]); c1 = T("c1", [128, 32]); ar = T("ar", [128, 32]); ai = T("ai", [128, 32])
                    def tt(out, a, b, op, rd, wr, eng="dve"):
                        k.op(eng, lambda e: e.tensor_tensor(out=out, in0=a, in1=b, op=op), reads=rd, writes=wr)
                    tt(mag[:], lre[:], dtt[:], ALU.mult, [lre, dtt], [mag])
                    k.op("act", lambda e: e.activation(out=dec8[:], in_=mag[:], func=AF.Exp, scale=8.0), reads=[mag], writes=[dec8])
                    k.op("act", lambda e: e.activation(out=mag[:], in_=mag[:], func=AF.Exp), reads=[mag], writes=[mag])
                    tt(th[:], lim[:], dtt[:], ALU.mult, [lim, dtt], [th])
                    k.op("dve", lambda e: e.tensor_scalar(out=thc[:], in0=th[:], scalar1=math.pi / 2, scalar2=None, op0=ALU.add), reads=[th], writes=[thc])
                    sin_red(s1, s1[:], th, th[:], (128, 32), tmps)
                    sin_red(c1, c1[:], thc, thc[:], (128, 32), tmps)
                    tt(ar[:], mag[:], c1[:], ALU.mult, [mag, c1], [ar])
                    tt(ai[:], mag[:], s1[:], ALU.mult, [mag, s1], [ai])
                    th8x = T("th8x", [128, 32])
                    k.op("dve", lambda e: e.tensor_scalar(out=th8x[:], in0=th[:], scalar1=8.0, scalar2=None, op0=ALU.mult), reads=[th], writes=[th8x])
                    jn = T("jn", [128, 32])
                    sin_red(jn, jn[:], th8x, th8x[:], (128, 32), tmps)
                    cp(k, "dve", th8[:], tr[:, 0:32], [tr], [th8])
                    den = T("den", [128, 32]); t1 = T("t1", [128, 32]); t2 = T("t2", [128, 32]); nr = T("nr", [128, 32])
                    cr = T("cr", [128, 32]); ci = T("ci", [128, 32])
                    tt(den[:], lre[:], lre[:], ALU.mult, [lre], [den])
                    tt(t1[:], lim[:], lim[:], ALU.mult, [lim], [t1])
                    tt(den[:], den[:], t1[:], ALU.add, [den, t1], [den])
                    k.op("dve", lambda e: e.reciprocal(out=den[:], in_=den[:]), reads=[den], writes=[den])
                    k.op("dve", lambda e: e.tensor_scalar(out=nr[:], in0=ar[:], scalar1=-1.0, scalar2=None, op0=ALU.add), reads=[ar], writes=[nr])
                    tt(t1[:], nr[:], lre[:], ALU.mult, [nr, lre], [t1])
                    tt(t2[:], ai[:], lim[:], ALU.mult, [ai, lim], [t2])
                    tt(t1[:], t1[:], t2[:], ALU.add, [t1, t2], [t1])
                    tt(cr[:], t1[:], den[:], ALU.mult, [t1, den], [cr])
                    tt(t1[:], ai[:], lre[:], ALU.mult, [ai, lre], [t1])
                    tt(t2[:], nr[:], lim[:], ALU.mult, [nr, lim], [t2])
                    tt(t1[:], t1[:], t2[:], ALU.subtract, [t1, t2], [t1])
                    tt(ci[:], t1[:], den[:], ALU.mult, [t1, den], [ci])
                    ir = T("ir", [128, 32]); ii = T("ii", [128, 32])
                    tt(t1[:], ar[:], ar[:], ALU.mult, [ar], [t1])
                    tt(t2[:], ai[:], ai[:], ALU.mult, [ai], [t2])
                    tt(t1[:], t1[:], t2[:], ALU.add, [t1, t2], [t1])
                    k.op("dve", lambda e: e.reciprocal(out=t1[:], in_=t1[:]), reads=[t1], writes=[t1])
                    tt(ir[:], ar[:], t1[:], ALU.mult, [ar, t1], [ir])
                    tt(ii[:], ai[:], t1[:], ALU.mult, [ai, t1], [ii])
                    k.op("dve", lambda e: e.tensor_scalar(out=ii[:], in0=ii[:], scalar1=-1.0, scalar2=None, op0=ALU.mult), reads=[ii], writes=[ii])
                    pTr = T("pTr", [128, 32, 9]); pTi = T("pTi", [128, 32, 9])
                    pNr = T("pNr", [128, 32, 8]); pNi = T("pNi", [128, 32, 8])
                    pAr = T("pAr", [128, 32, 8]); pAi = T("pAi", [128, 32, 8])
                    for (pr_, pi_) in ((pTr, pTi), (pNr, pNi)):
                        k.op("dve", lambda e, pr_=pr_: e.memset(pr_[:, :, 0:1], 1.0), reads=[pr_], writes=[pr_])
                        k.op("dve", lambda e, pi_=pi_: e.memset(pi_[:, :, 0:1], 0.0), reads=[pi_], writes=[pi_])
                    def cmul(pr_, pi_, kk, mr, mi):
                        tt(t1[:], pr_[:, :, kk], mr[:], ALU.mult, [pr_, mr], [t1])
                        tt(t2[:], pi_[:, :, kk], mi[:], ALU.mult, [pi_, mi], [t2])
                        tt(pr_[:, :, kk + 1], t1[:], t2[:], ALU.subtract, [t1, t2, pr_], [pr_])
                        tt(t1[:], pr_[:, :, kk], mi[:], ALU.mult, [pr_, mi], [t1])
                        tt(t2[:], pi_[:, :, kk], mr[:], ALU.mult, [pi_, mr], [t2])
                        tt(pi_[:, :, kk + 1], t1[:], t2[:], ALU.add, [t1, t2, pi_], [pi_])
                    for kk in range(8):
                        cmul(pTr, pTi, kk, ar, ai)
                    for kk in range(7):
                        cmul(pNr, pNi, kk, ir, ii)
                    for s_ in range(8):
                        cp(k, "dve", pAr[:, :, s_], pTr[:, :, 7 - s_], [pTr, pAr], [pAr])
                        cp(k, "dve", pAi[:, :, s_], pTi[:, :, 7 - s_], [pTi, pAi], [pAi])
                    Bre = T("Bre", [128, 32, 16]); Bim = T("Bim", [128, 32, 16])
                    for gh in range(2):
                        k.dma("sp", [(Bre[gh * 64:(gh + 1) * 64], ssm_b_re[gh * 32:(gh + 1) * 32].rearrange("g p h -> p g h"))], Bre, reads=[Bre], writes=[Bre])
                        k.dma("sp", [(Bim[gh * 64:(gh + 1) * 64], ssm_b_im[gh * 32:(gh + 1) * 32].rearrange("g p h -> p g h"))], Bim, reads=[Bim], writes=[Bim])
                    Bbr = T("Bbr", [128, 32, 16]); Bbi = T("Bbi", [128, 32, 16]); tb1 = T("tb1", [128, 32, 16]); tb2 = T("tb2", [128, 32, 16])
                    def bc16(x):
                        return x[:].unsqueeze(2).to_broadcast([128, 32, 16])
                    tt(tb1[:], Bre[:], bc16(cr), ALU.mult, [Bre, cr], [tb1])
                    tt(tb2[:], Bim[:], bc16(ci), ALU.mult, [Bim, ci], [tb2])
                    tt(Bbr[:], tb1[:], tb2[:], ALU.subtract, [tb1, tb2], [Bbr])
                    tt(tb1[:], Bim[:], bc16(cr), ALU.mult, [Bim, cr], [tb1])
                    tt(tb2[:], Bre[:], bc16(ci), ALU.mult, [Bre, ci], [tb2])
                    tt(Bbi[:], tb1[:], tb2[:], ALU.add, [tb1, tb2], [Bbi])
                    CrT = T("CrT", [128, 32, 16]); CiT = T("CiT", [128, 32, 16])
                    cst = T("cst", [128, 128]); pcs = k.ps("pcs", [128, 128], F32)
                    for (src, dstT) in ((ssm_c_re, CrT), (ssm_c_im, CiT)):
                        for q in range(4):
                            k.dma("sp", [(cst[:, 0:64], src[8 * q:8 * q + 8].rearrange("g h p -> (g h) p")),
                                         (cst[:, 64:128], src[32 + 8 * q:32 + 8 * q + 8].rearrange("g h p -> (g h) p"))], cst, reads=[cst], writes=[cst])
                            k.op("pe", lambda e: e.transpose(out=pcs[:], in_=cst[:], identity=identf[:]), reads=[cst, identf], writes=[pcs])
                            cp(k, "dve", dstT[:, 8 * q:8 * q + 8, :], pcs[:].rearrange("p (g h) -> p g h", h=16), [pcs], [dstT])
                    dcol = T("dcol", [128, 64])
                    with nc.allow_non_contiguous_dma(reason="tiny transposed parameter loads"):
                        for s_ in range(8):
                            k.dma("sp", [(dcol[s_ * 16:(s_ + 1) * 16, :], ssm_d.rearrange("g h -> h g"))], dcol, reads=[dcol], writes=[dcol])
                        k.flush()
                    maskM = T("maskM", [128, 128])
                    k.op("pool", lambda e: e.memset(maskM[:], 1.0), writes=[maskM])
                    k.op("pool", lambda e: e.affine_select(out=maskM[:].rearrange("p (t h) -> p t h", h=16), in_=maskM[:].rearrange("p (t h) -> p t h", h=16), pattern=[[16, 8], [0, 16]],
                                                           compare_op=ALU.is_ge, fill=0.0, base=15, channel_multiplier=-1), reads=[maskM], writes=[maskM])
                    HG = 2
                    B7r = T("B7r", [128, HG, 8, 16]); B7i = T("B7i", [128, HG, 8, 16]); BNr = T("BNr", [128, HG, 8, 16]); BNi = T("BNi", [128, HG, 8, 16])
                    Ctr = T("Ctr", [128, HG, 8, 16]); Cti = T("Cti", [128, HG, 8, 16]); Qr = T("Qr", [128, HG, 8, 16]); Qi = T("Qi", [128, HG, 8, 16])
                    X1 = T("X1", [128, HG, 8, 16]); X2 = T("X2", [128, HG, 8, 16])
                    pM = [k.ps("pM%d" % i, [128, 128], F32) for i in range(2)]
                    pP = [k.ps("pP%d" % i, [128, 128], F32) for i in range(2)]
                    mt1 = T("mt1", [128, 128]); mt2 = T("mt2", [128, 128])
                    for half in range(32 // HG):
                        gs = slice(half * HG, (half + 1) * HG)
                        def pw(tbl, lo, hi):
                            return tbl[:, gs, lo:hi].unsqueeze(3).to_broadcast([128, HG, 8, 16])
                        def vb(tbl):
                            return tbl[:, gs, :].unsqueeze(2).to_broadcast([128, HG, 8, 16])
                        def cprod(outr, outi, pr_, pi_, lo, hi, vr, vi, neg_im, eng="dve"):
                            tt(X1[:], pw(pr_, lo, hi), vb(vr), ALU.mult, [pr_, vr], [X1], eng)
                            tt(X2[:], pw(pi_, lo, hi), vb(vi), ALU.mult, [pi_, vi], [X2], eng)
                            tt(outr[:], X1[:], X2[:], ALU.subtract, [X1, X2], [outr], eng)
                            tt(X1[:], pw(pr_, lo, hi), vb(vi), ALU.mult, [pr_, vi], [X1], eng)
                            tt(X2[:], pw(pi_, lo, hi), vb(vr), ALU.mult, [pi_, vr], [X2], eng)
                            tt(outi[:], X1[:], X2[:], ALU.add, [X1, X2], [outi], eng)
                            if neg_im:
                                k.op(eng, lambda e: e.tensor_scalar(out=outi[:], in0=outi[:], scalar1=-1.0, scalar2=None, op0=ALU.mult), reads=[outi], writes=[outi])
                        cprod(B7r, B7i, pAr, pAi, 0, 8, Bbr, Bbi, False)
                        cprod(BNr, BNi, pNr, pNi, 0, 8, Bbr, Bbi, False)
                        cprod(Ctr, Cti, pTr, pTi, 0, 8, CrT, CiT, True)
                        cprod(Qr, Qi, pTr, pTi, 1, 9, CrT, CiT, True)
                        cp(k, "dve", QmRe[:, gs, :], Qr[:].rearrange("p g t h -> p g (t h)"), [Qr], [QmRe])
                        cp(k, "dve", QmIm[:, gs, :], Qi[:].rearrange("p g t h -> p g (t h)"), [Qi], [QmIm])
                        for gl in range(HG):
                            gi = half * HG + gl
                            for (srcB, dstP, pp_) in ((B7r, PmRe, pP[0]), (B7i, PmIm, pP[1])):
                                k.op("pe", lambda e, srcB=srcB, pp_=pp_, gl=gl: e.transpose(out=pp_[:], in_=srcB[:, gl].rearrange("p s h -> p (s h)"), identity=identf[:]), reads=[srcB, identf], writes=[pp_])
                                cp(k, "act", dstP[:, gi, :], pp_[:], [pp_], [dstP])
                            for gh in range(2):
                                g = gh * 32 + gi
                                pm_ = pM[gh]
                                rs = slice(gh * 64, (gh + 1) * 64)
                                k.op("pe", lambda e, pm_=pm_, rs=rs, gl=gl: e.matmul(pm_[:], lhsT=BNr[rs, gl].rearrange("p s h -> p (s h)"), rhs=Ctr[rs, gl].rearrange("p s h -> p (s h)"), start=True, stop=False), reads=[BNr, Ctr], writes=[pm_], inc=False)
                                k.op("pe", lambda e, pm_=pm_, rs=rs, gl=gl: e.matmul(pm_[:], lhsT=BNi[rs, gl].rearrange("p s h -> p (s h)"), rhs=Cti[rs, gl].rearrange("p s h -> p (s h)"), start=False, stop=True), reads=[BNi, Cti], writes=[pm_])
                                tt(mt1[:], pm_[:], maskM[:], ALU.mult, [pm_, maskM], [mt1])
                                k.op("dve", lambda e, g=g: e.scalar_tensor_tensor(out=mt2[:], in0=identf[:], scalar=dcol[:, g:g + 1], in1=mt1[:], op0=ALU.mult, op1=ALU.add), reads=[identf, dcol, mt1], writes=[mt2])
                                cp(k, "dve", Mm[:, g, :], mt2[:], [mt2], [Mm])

            with k.phase():
                tq = k.sb("tq", [128, 512], F32); ti = k.sb("ti", [128, 512], I32); tr = k.sb("tr", [128, 512], F32); tm = k.sb("tm", [128, 512], F32)
                tmps = (tq, ti, tr, tm)
                ang = k.sb("ang", [128, 512], F32); angc = k.sb("angc", [128, 512], F32)
                cosT = k.sb("cosT", [128, 512], F32); sinT = k.sb("sinT", [128, 512], F32)
                u2 = [k.sb("u2_%d" % i, [128, 2, 512], BF16) for i in range(2)]
                Vre = k.ps("Vre", [128, 512], F32); Vim = k.ps("Vim", [128, 512], F32)
                Yp = [k.ps("Yp%d" % i, [128, 4, 128], F32) for i in range(2)]
                a1 = k.sb("a1", [128, 512], F32); a2 = k.sb("a2", [128, 512], F32); a3 = k.sb("a3", [128, 512], F32); a4 = k.sb("a4", [128, 512], F32)
                wri = k.sb("wri", [128, 512], F32); wii = k.sb("wii", [128, 512], F32)
                wr_ = k.sb("wr_", [128, 512], F32); wi_ = k.sb("wi_", [128, 512], F32)
                Xr = [k.sb("Xr%d" % i, [128, 520], BF16) for i in range(2)]
                Xi = [k.sb("Xi%d" % i, [128, 520], BF16) for i in range(2)]
                uo = k.sb("uo", [128, 2, 256], BF16); xro = k.sb("xro", [128, 256], BF16); xio = k.sb("xio", [128, 256], BF16)
                for i in range(2):
                    k.op("pool", lambda e, i=i: e.memset(Xr[i][:], 0.0), writes=[Xr[i]])
                    k.op("pool", lambda e, i=i: e.memset(Xi[i][:], 0.0), writes=[Xi[i]])
                def tt(out, a, b, op, rd, wr, eng="dve"):
                    k.op(eng, lambda e: e.tensor_tensor(out=out, in0=a, in1=b, op=op), reads=rd, writes=wr)
                for gi in range(32):
                    sl_ = gi % 2
                    uu = u2[sl_]
                    k.dma("sp", [(uu[:, gh, :], U_d[:, :, gh * 32 + gi, :].rearrange("s h c -> (s h) c")) for gh in range(2)], uu, reads=[U_d], writes=[uu])
                    k.op("dve", lambda e, gi=gi: e.tensor_scalar(out=ang[:], in0=iot[:], scalar1=th8[:, gi:gi + 1], scalar2=None, op0=ALU.mult), reads=[iot, th8], writes=[ang])
                    k.op("pool", lambda e: e.tensor_scalar(out=angc[:], in0=ang[:], scalar1=math.pi / 2, scalar2=None, op0=ALU.add), reads=[ang], writes=[angc])
                    sin_red(sinT, sinT[:], ang, ang[:], (128, 512), tmps)
                    sin_red(cosT, cosT[:], angc, angc[:], (128, 512), tmps)
                    for gh in range(2):
                        rs = slice(gh * 64, (gh + 1) * 64)
                        k.op("pe", lambda e, rs=rs, gi=gi, gh=gh, uu=uu: e.matmul(Vre[rs, :], lhsT=PmRe[:, gi, rs], rhs=uu[:, gh, :], start=True, stop=True), reads=[PmRe, uu], writes=[Vre], inc=(gh == 1))
                    for gh in range(2):
                        rs = slice(gh * 64, (gh + 1) * 64)
                        k.op("pe", lambda e, rs=rs, gi=gi, gh=gh, uu=uu: e.matmul(Vim[rs, :], lhsT=PmIm[:, gi, rs], rhs=uu[:, gh, :], start=True, stop=True), reads=[PmIm, uu], writes=[Vim], inc=(gh == 1))
                    tt(a1[:], Vre[:], cosT[:], ALU.mult, [Vre, cosT], [a1])
                    tt(a2[:], Vim[:], sinT[:], ALU.mult, [Vim, sinT], [a2])
                    tt(a3[:], Vim[:], cosT[:], ALU.mult, [Vim, cosT], [a3])
                    tt(a4[:], Vre[:], sinT[:], ALU.mult, [Vre, sinT], [a4])
                    tt(wri[:], a1[:], a2[:], ALU.add, [a1, a2], [wri], "pool")
                    tt(wii[:], a3[:], a4[:], ALU.subtract, [a3, a4], [wii], "pool")
                    dbc = dec8[:, gi:gi + 1].to_broadcast([128, 512])
                    k.op("dve", lambda e, dbc=dbc: e.tensor_tensor_scan(out=wr_[:], data0=dbc, data1=wri[:], initial=0.0, op0=ALU.mult, op1=ALU.add), reads=[dec8, wri], writes=[wr_])
                    k.op("dve", lambda e, dbc=dbc: e.tensor_tensor_scan(out=wi_[:], data0=dbc, data1=wii[:], initial=0.0, op0=ALU.mult, op1=ALU.add), reads=[dec8, wii], writes=[wi_])
                    xr_, xi_ = Xr[sl_], Xi[sl_]
                    tt(a1[:], wr_[:], cosT[:], ALU.mult, [wr_, cosT], [a1], "pool")
                    tt(a2[:], wi_[:], sinT[:], ALU.mult, [wi_, sinT], [a2], "pool")
                    tt(xr_[:, 1:513], a1[:], a2[:], ALU.subtract, [a1, a2], [xr_], "pool")
                    tt(a3[:], wi_[:], cosT[:], ALU.mult, [wi_, cosT], [a3], "pool")
                    tt(a4[:], wr_[:], sinT[:], ALU.mult, [wr_, sinT], [a4], "pool")
                    tt(xi_[:, 1:513], a3[:], a4[:], ALU.add, [a3, a4], [xi_], "pool")
                    def sel(dst, dstb, src_ap, srcb, eng1, eng2):
                        v = src_ap
                        k.op(eng1, lambda e: e.tensor_scalar(out=dst, in0=v[0], scalar1=ccs[:, 0:1], scalar2=None, op0=ALU.mult), reads=[srcb, ccs], writes=[dstb])
                        k.op(eng2, lambda e: e.scalar_tensor_tensor(out=dst, in0=v[1], scalar=ccs[:, 1:2], in1=dst, op0=ALU.mult, op1=ALU.add), reads=[srcb, ccs, dstb], writes=[dstb])
                    uv = uu[:].rearrange("p g (j r i) -> p g j r i", r=2, i=16)
                    sel(uo[:].rearrange("p g (j i) -> p g j i", i=16), uo, (uv[:, :, :, 0, :], uv[:, :, :, 1, :]), uu, "pool", "dve")
                    xv = xr_[:, 0:512].rearrange("p (j r i) -> p j r i", r=2, i=16)
                    sel(xro[:].rearrange("p (j i) -> p j i", i=16), xro, (xv[:, :, 0, :], xv[:, :, 1, :]), xr_, "pool", "dve")
                    xv2 = xi_[:, 0:512].rearrange("p (j r i) -> p j r i", r=2, i=16)
                    sel(xio[:].rearrange("p (j i) -> p j i", i=16), xio, (xv2[:, :, 0, :], xv2[:, :, 1, :]), xi_, "pool", "dve")
                    yp = Yp[gi % 2]
                    for gh in range(2):
                        g = gh * 32 + gi
                        rs = slice(gh * 64, (gh + 1) * 64)
                        for cbk in range(2):
                            slot = gh * 2 + cbk
                            cs = slice(cbk * 128, (cbk + 1) * 128)
                            k.op("pe", lambda e, yp=yp, slot=slot, gh=gh, cs=cs, g=g: e.matmul(yp[:, slot, :], lhsT=uo[:, gh, cs], rhs=Mm[:, g, :], start=True, stop=False), reads=[uo, Mm], writes=[yp], inc=False)
                            k.op("pe", lambda e, yp=yp, slot=slot, rs=rs, cs=cs, gi=gi: e.matmul(yp[:, slot, :], lhsT=xro[rs, cs], rhs=QmRe[rs, gi, :], start=False, stop=False), reads=[xro, QmRe], writes=[yp], inc=False)
                            k.op("pe", lambda e, yp=yp, slot=slot, rs=rs, cs=cs, gi=gi: e.matmul(yp[:, slot, :], lhsT=xio[rs, cs], rhs=QmIm[rs, gi, :], start=False, stop=True), reads=[xio, QmIm], writes=[yp], inc=(slot == 3))
                    for gh in range(2):
                        g = gh * 32 + gi
                        for cbk in range(2):
                            slot = gh * 2 + cbk
                            k.op("act", lambda e, yp=yp, slot=slot, g=g, cbk=cbk: e.copy(out=y_tm[cbk][:, :, g * 16:(g + 1) * 16], in_=yp[:, slot, :].rearrange("p (t h) -> p t h", h=16)),
                                 reads=[yp], writes=[y_tm[cbk]])
            wphase.__exit__(None, None, None)
            if "y" in dbg:
                o = outp("dbg_y", [2, 128, 8 * 1024], F32)
                for cbk in range(2):
                    k.dma("sp", [(o[cbk], y_tm[cbk][:].rearrange("p t c -> p (t c)"))], y_tm[cbk], reads=[y_tm[cbk]])
            with k.phase():
                wg = k.sb("wg", [128, 8, 1024], BF16)
                k.dma("pool", [(wg[:], w_glu.rearrange("(kc p) n -> p kc n", p=128))], wg, writes=[wg])
                bg = k.sb("bg", [128, 1024], F32)
                k.dma("sp", [(bg[:], b_glu.partition_broadcast(128))], bg, writes=[bg])
                gn = k.sb("gn", [128, 1024], F32)
                k.dma("sp", [(gn[:], ssm_norm_g.partition_broadcast(128))], gn, writes=[gn])
                x2 = k.sb("x2", [128, 1024], F32); inn = k.sb("inn", [128, 1024], F32); sg_ = k.sb("sg_", [128, 1024], F32)
                sf = k.sb("sf", [128, 1024], F32); sbf = k.sb("sbf", [128, 1024], BF16)
                sT = k.sb("sT", [128, 8, 128], BF16)
                gt = k.sb("gt", [128, 1024], F32); s2 = k.sb("s2", [128, 1024], F32); s3 = k.sb("s3", [128, 1024], BF16)
                jk2 = k.sb("jk2", [128, 1024], BF16); ms2 = k.sb("ms2", [128, 1], F32)
                catS = k.sb("catS", [128, 8, 1024], BF16)
                pTs = k.ps("pTs", [128, 8, 128], BF16)
                pG = [k.ps("pG%d" % i, [128, 512], F32) for i in range(2)]
                C0 = 2.0 * math.sqrt(2.0 / math.pi)
                for cbk in range(2):
                    for t in range(8):
                        yv = y_tm[cbk][:, t, :]
                        k.op("pool", lambda e, yv=yv: e.tensor_tensor(out=x2[:], in0=yv, in1=yv, op=ALU.mult), reads=[y_tm[cbk]], writes=[x2])
                        k.op("dve", lambda e: e.tensor_scalar(out=x2[:], in0=x2[:], scalar1=0.044715, scalar2=1.0, op0=ALU.mult, op1=ALU.add), reads=[x2], writes=[x2])
                        k.op("pool", lambda e, yv=yv: e.tensor_tensor(out=inn[:], in0=x2[:], in1=yv, op=ALU.mult), reads=[x2, y_tm[cbk]], writes=[inn])
                        k.op("act", lambda e: e.activation(out=sg_[:], in_=inn[:], func=AF.Sigmoid, scale=C0), reads=[inn], writes=[sg_])
                        k.op("dve", lambda e, yv=yv: e.tensor_tensor(out=sf[:], in0=sg_[:], in1=yv, op=ALU.mult), reads=[sg_, y_tm[cbk]], writes=[sf])
                        cp(k, "pool", sbf[:], sf[:], [sf], [sbf])
                        for kc in range(8):
                            k.op("pe", lambda e, kc=kc: e.transpose(out=pTs[:, kc, :], in_=sbf[:, kc * 128:(kc + 1) * 128], identity=ident[:]), reads=[sbf, ident], writes=[pTs], inc=(kc == 7))
                        cp(k, "act", sT[:], pTs[:], [pTs], [sT])
                        for half in range(2):
                            for kc in range(8):
                                k.op("pe", lambda e, half=half, kc=kc: e.matmul(pG[half][:], lhsT=sT[:, kc, :], rhs=wg[:, kc, half * 512:(half + 1) * 512], start=(kc == 0), stop=(kc == 7)),
                                     reads=[sT, wg], writes=[pG[half]], inc=(kc == 7))
                            k.op("dve", lambda e, half=half: e.tensor_tensor(out=gt[:, half * 512:(half + 1) * 512], in0=pG[half][:], in1=bg[:, half * 512:(half + 1) * 512], op=ALU.add), reads=[pG[half], bg], writes=[gt])
                        k.op("act", lambda e: e.activation(out=gt[:], in_=gt[:], func=AF.Sigmoid), reads=[gt], writes=[gt])
                        k.op("dve", lambda e: e.tensor_tensor(out=s2[:], in0=gt[:], in1=sf[:], op=ALU.mult), reads=[gt, sf], writes=[s2])
                        k.op("act", lambda e: e.activation(out=jk2[:], in_=s2[:], func=AF.Square, accum_out=ms2[:]), reads=[s2], writes=[jk2, ms2])
                        k.op("dve", lambda e: e.tensor_scalar(out=ms2[:], in0=ms2[:], scalar1=1.0 / 1024, scalar2=EPS, op0=ALU.mult, op1=ALU.add), reads=[ms2], writes=[ms2])
                        k.op("act", lambda e: e.activation(out=ms2[:], in_=ms2[:], func=AF.Sqrt), reads=[ms2], writes=[ms2])
                        k.op("dve", lambda e: e.reciprocal(out=ms2[:], in_=ms2[:]), reads=[ms2], writes=[ms2])
                        k.op("dve", lambda e: e.scalar_tensor_tensor(out=s3[:], in0=s2[:], scalar=ms2[:], in1=gn[:], op0=ALU.mult, op1=ALU.mult), reads=[s2, ms2, gn], writes=[s3])
                        for kc in range(8):
                            k.op("pe", lambda e, kc=kc: e.transpose(out=pTs[:, kc, :], in_=s3[:, kc * 128:(kc + 1) * 128], identity=ident[:]), reads=[s3, ident], writes=[pTs], inc=(kc == 7))
                        cp(k, "act", catS[:, :, t * 128:(t + 1) * 128], pTs[:], [pTs], [catS])
                    k.dma("sp", [(catT_d[8:16, :, cbk * 1024:(cbk + 1) * 1024].rearrange("c p n -> p c n"), catS[:])], catS, reads=[catS], writes=[catT_d])
        if "s" in dbg:
            with k.phase():
                o = outp("dbg_sT", [8, 128, NOWN], BF16)
                tmp = k.sb("dbgts", [128, 8, NOWN], BF16)
                k.dma("sp", [(tmp[:], catT_d[8:16].rearrange("h p n -> p h n"))], tmp, reads=[catT_d], writes=[tmp])
                k.dma("sp", [(o.rearrange("h p n -> p h n"), tmp[:])], tmp, reads=[tmp])

        if upto >= 5:
          with k.phase():
            wo = k.sb("wo", [128, 16, D], BF16)
            w_ov = w_o.rearrange("(kc p) n -> p kc n", p=128)
            k.dma("pool", [(wo[:, 4 * q:4 * q + 4, :], w_ov[:, 4 * q:4 * q + 4, :]) for q in range(4)], wo, writes=[wo])
            gf = k.sb("gf", [128, D], F32)
            k.dma("sp", [(gf[:], g_ffn.partition_broadcast(128))], gf, writes=[gf])
            wrs = k.sb("wrs", [128, 16, 36], F32)
            k.dma("sp", [(wrs[:], wr.rearrange("(kc p) n -> p kc n", p=128))], wrs, writes=[wrs])
            brb = k.sb("brb", [128, 36], F32)
            k.dma("sp", [(brb[:], br.partition_broadcast(128))], brb, writes=[brb])
            Ltri = k.sb("Ltri", [128, 128], BF16); onesb = k.sb("onesb", [128, 128], BF16); ltf = k.sb("ltf", [128, 128], F32)
            k.op("pool", lambda e: e.memset(ltf[:], 1.0), writes=[ltf])
            k.op("pool", lambda e: e.affine_select(out=ltf[:], in_=ltf[:], pattern=[[1, 128]], compare_op=ALU.is_ge, fill=0.0, base=-1, channel_multiplier=-1), reads=[ltf], writes=[ltf])
            cp(k, "dve", Ltri[:], ltf[:], [ltf], [Ltri])
            k.op("pool", lambda e: e.memset(onesb[:], 1.0), writes=[onesb])
            iote = k.sb("iote", [128, 32], F32); iotei = k.sb("iotei", [128, 32], I32)
            k.op("pool", lambda e: e.iota(iotei[:], pattern=[[1, 32]], base=0, channel_multiplier=0), writes=[iotei])
            cp(k, "dve", iote[:], iotei[:], [iotei], [iote])
            cntb = k.sb("cntb", [128, 32], F32)
            k.op("dve", lambda e: e.memset(cntb[:], 0.0), writes=[cntb])
            aTs = k.sb("aTs", [128, 8, 1024], BF16); sTs = k.sb("sTs", [128, 8, 1024], BF16)
            xot = [k.sb("xot%d" % i, [128, D], F32) for i in range(2)]
            x1t = [k.sb("x1t%d" % i, [128, D], F32) for i in range(2)]
            h2f = k.sb("h2f", [128, D], F32); h2b = [k.sb("h2b%d" % i, [128, D], BF16) for i in range(2)]
            jk5 = k.sb("jk5", [128, D], BF16); ms5 = k.sb("ms5", [128, 1], F32)
            h2T = k.sb("h2T", [128, 16, 128], F32)
            pO = [k.ps("pO%d" % i, [128, 512], F32) for i in range(4)]
            pX = [k.ps("pX%d" % i, [128, 4, 128], F32) for i in range(2)]
            pR = k.ps("pR", [128, 512], F32)
            def S_(name, n, dt=F32):
                return k.sb(name, [128, n], dt)
            Lg = S_("Lg", 36); mg = S_("mg", 1); nmg = S_("nmg", 1); ohg = S_("ohg", 4); eg = S_("eg", 4); zg = S_("zg", 1); gate = S_("gate", 1)
            tm3 = k.sb("tm3", [128, 8, 4], F32); les = S_("les", 8); m1 = S_("m1", 1); oh1 = S_("oh1", 8); les2 = S_("les2", 8); m2 = S_("m2", 1); oh2 = S_("oh2", 8)
            dd = S_("dd", 1); e2 = S_("e2", 1); rden = S_("rden", 1)
            OH = [k.sb("OH%d" % i, [128, 4, 8], F32) for i in range(2)]
            posk = k.sb("posk", [128, 16, 2], F32); eidk = k.sb("eidk", [128, 16, 2], F32)
            Ab = S_("Ab", 32, BF16); posf = S_("posf", 32); t32 = S_("t32", 32); pk = S_("pk", 1); ek = S_("ek", 1); ov = S_("ov", 1); df = S_("df", 1)
            def ts(out, a, s1, s2, o0, o1, rd, wr_):
                k.op("dve", lambda e: e.tensor_scalar(out=out, in0=a, scalar1=s1, scalar2=s2, op0=o0, **({"op1": o1} if o1 is not None else {})), reads=rd, writes=wr_)
            def tt(out, a, b, op, rd, wr_, eng="dve"):
                k.op(eng, lambda e: e.tensor_tensor(out=out, in0=a, in1=b, op=op), reads=rd, writes=wr_)
            def red(out, a, op, rd, wr_):
                k.op("dve", lambda e: e.tensor_reduce(out=out, in_=a, axis=AX.X, op=op), reads=rd, writes=wr_)
            for it in range(16):
                cbk, t = it // 8, it % 8
                if t == 0:
                    k.dma("sp", [(aTs[:], catT_d[0:8, :, cbk * 1024:(cbk + 1) * 1024].rearrange("c p n -> p c n"))], aTs, reads=[catT_d], writes=[aTs])
                    k.dma("sp", [(sTs[:], catT_d[8:16, :, cbk * 1024:(cbk + 1) * 1024].rearrange("c p n -> p c n"))], sTs, reads=[catT_d], writes=[sTs])
                xs = xot[it % 2]; x1 = x1t[it % 2]; hb_ = h2b[it % 2]
                k.dma("act", [(xs[:], xo[it * 128:(it + 1) * 128, :])], xs, writes=[xs])
                aview = aTs[:].rearrange("p h (c t) -> p h c t", t=8)
                for nb in range(4):
                    pp = pO[nb]
                    for kc in range(16):
                        lhs = aview[:, kc, :, t] if kc < 8 else sTs[:, kc - 8, t * 128:(t + 1) * 128]
                        rdb = aTs if kc < 8 else sTs
                        k.op("pe", lambda e, pp=pp, lhs=lhs, kc=kc, nb=nb: e.matmul(pp[:], lhsT=lhs, rhs=wo[:, kc, nb * 512:(nb + 1) * 512], start=(kc == 0), stop=(kc == 15)),
                             reads=[rdb, wo], writes=[pp], inc=(kc == 15))
                    tt(x1[:, nb * 512:(nb + 1) * 512], pp[:], xs[:, nb * 512:(nb + 1) * 512], ALU.add, [pp, xs], [x1])
                k.dma("act", [(X1_d[it * 128:(it + 1) * 128, :], x1[:])], x1, reads=[x1], writes=[X1_d])
                k.op("act", lambda e, x1=x1: e.activation(out=jk5[:], in_=x1[:], func=AF.Square, accum_out=ms5[:]), reads=[x1], writes=[jk5, ms5])
                ts(ms5[:], ms5[:], 1.0 / D, EPS, ALU.mult, ALU.add, [ms5], [ms5])
                k.op("act", lambda e: e.activation(out=ms5[:], in_=ms5[:], func=AF.Sqrt), reads=[ms5], writes=[ms5])
                k.op("dve", lambda e: e.reciprocal(out=ms5[:], in_=ms5[:]), reads=[ms5], writes=[ms5])
                k.op("dve", lambda e, x1=x1: e.scalar_tensor_tensor(out=h2f[:], in0=x1[:], scalar=ms5[:], in1=gf[:], op0=ALU.mult, op1=ALU.mult), reads=[x1, ms5, gf], writes=[h2f])
                cp(k, "pool", hb_[:], h2f[:], [h2f], [hb_])
                for q in range(4):
                    px = pX[q % 2]
                    for c in range(4):
                        kc = q * 4 + c
                        k.op("pe", lambda e, px=px, c=c, kc=kc: e.transpose(out=px[:, c, :], in_=h2f[:, kc * 128:(kc + 1) * 128], identity=identf[:]), reads=[h2f, identf], writes=[px], inc=(c == 3))
                    cp(k, "act", h2T[:, q * 4:(q + 1) * 4, :], px[:], [px], [h2T])
                for kc in range(16):
                    k.op("pe", lambda e, kc=kc: e.matmul(pR[:, 0:36], lhsT=h2T[:, kc, :], rhs=wrs[:, kc, :], start=(kc == 0), stop=(kc == 15)), reads=[h2T, wrs], writes=[pR], inc=(kc == 15))
                tt(Lg[:], pR[:, 0:36], brb[:], ALU.add, [pR, brb], [Lg])
                red(mg[:], Lg[:, 0:4], ALU.max, [Lg], [mg])
                ts(ohg[:], Lg[:, 0:4], mg[:, 0:1], None, ALU.is_equal, None, [Lg, mg], [ohg])
                ts(nmg[:], mg[:], -1.0, None, ALU.mult, None, [mg], [nmg])
                k.op("act", lambda e: e.activation(out=eg[:], in_=Lg[:, 0:4], func=AF.Exp, bias=nmg[:], scale=1.0, accum_out=zg[:]), reads=[Lg, nmg], writes=[eg, zg])
                k.op("dve", lambda e: e.reciprocal(out=gate[:], in_=zg[:]), reads=[zg], writes=[gate])
                tt(tm3[:], Lg[:, 4:36].rearrange("p (g e) -> p e g", g=4), ohg[:].unsqueeze(1).to_broadcast([128, 8, 4]), ALU.mult, [Lg, ohg], [tm3])
                red(les[:], tm3[:], ALU.add, [tm3], [les])
                red(m1[:], les[:], ALU.max, [les], [m1])
                ts(oh1[:], les[:], m1[:, 0:1], None, ALU.is_equal, None, [les, m1], [oh1])
                k.op("dve", lambda e: e.scalar_tensor_tensor(out=les2[:], in0=oh1[:], scalar=-1e30, in1=les[:], op0=ALU.mult, op1=ALU.add), reads=[oh1, les], writes=[les2])
                red(m2[:], les2[:], ALU.max, [les2], [m2])
                ts(oh2[:], les2[:], m2[:, 0:1], None, ALU.is_equal, None, [les2, m2], [oh2])
                tt(dd[:], m2[:], m1[:], ALU.subtract, [m2, m1], [dd])
                k.op("act", lambda e: e.activation(out=e2[:], in_=dd[:], func=AF.Exp), reads=[dd], writes=[e2])
                ts(rden[:], e2[:], 1.0, None, ALU.add, None, [e2], [rden])
                k.op("dve", lambda e: e.reciprocal(out=rden[:], in_=rden[:]), reads=[rden], writes=[rden])
                tt(wts[:, it, 0:1], gate[:], rden[:], ALU.mult, [gate, rden, wts], [wts])
                tt(wts[:, it, 1:2], wts[:, it, 0:1], e2[:], ALU.mult, [wts, e2], [wts])
                for kk, ohk in enumerate((oh1, oh2)):
                    tt(OH[kk][:], ohg[:].unsqueeze(2).to_broadcast([128, 4, 8]), ohk[:].unsqueeze(1).to_broadcast([128, 4, 8]), ALU.mult, [ohg, ohk], [OH[kk]])
                tt(Ab[:], OH[0][:].rearrange("p g e -> p (g e)"), OH[1][:].rearrange("p g e -> p (g e)"), ALU.add, [OH[0], OH[1]], [Ab])
                k.op("pe", lambda e: e.matmul(pR[:, 64:96], lhsT=Ltri[:], rhs=Ab[:], start=True, stop=True), reads=[Ltri, Ab], writes=[pR], inc=False)
                k.op("pe", lambda e: e.matmul(pR[:, 128:160], lhsT=onesb[:], rhs=Ab[:], start=True, stop=True), reads=[onesb, Ab], writes=[pR])
                tt(posf[:], pR[:, 64:96], cntb[:], ALU.add, [pR, cntb], [posf])
                tt(cntb[:], pR[:, 128:160], cntb[:], ALU.add, [pR, cntb], [cntb])
                for kk in range(2):
                    ohf = OH[kk][:].rearrange("p g e -> p (g e)")
                    tt(t32[:], ohf, posf[:], ALU.mult, [OH[kk], posf], [t32])
                    red(pk[:], t32[:], ALU.add, [t32], [pk])
                    tt(t32[:], ohf, iote[:], ALU.mult, [OH[kk], iote], [t32])
                    red(ek[:], t32[:], ALU.add, [t32], [ek])
                    cp(k, "dve", posk[:, it, kk:kk + 1], pk[:], [pk, posk], [posk])
                    cp(k, "dve", eidk[:, it, kk:kk + 1], ek[:], [ek, eidk], [eidk])
                k.dma("act", [(H2_d[it * 128:(it + 1) * 128, :], hb_[:])], hb_, reads=[hb_], writes=[H2_d])
            t3i = k.sb("t3i", [128, 32, 32], I32); t3a = k.sb("t3a", [128, 32, 32], F32); t3b = k.sb("t3b", [128, 32, 32], F32)
            k.op("pool", lambda e: e.iota(t3i[:], pattern=[[0, 32], [128, 32]], base=0, channel_multiplier=0), writes=[t3i])
            cp(k, "dve", t3a[:], t3i[:], [t3i], [t3a])
            nblk = S_("nblk", 32); cumb = S_("cumb", 32); pstart = S_("pstart", 32); ones32 = S_("ones32", 32)
            tt(t3b[:], cntb[:].unsqueeze(2).to_broadcast([128, 32, 32]), t3a[:], ALU.is_gt, [cntb, t3a], [t3b])
            red(nblk[:], t3b[:], ALU.add, [t3b], [nblk])
            k.op("dve", lambda e: e.memset(ones32[:], 1.0), writes=[ones32])
            k.op("dve", lambda e: e.tensor_tensor_scan(out=cumb[:], data0=ones32[:], data1=nblk[:], initial=0.0, op0=ALU.mult, op1=ALU.add), reads=[ones32, nblk], writes=[cumb])
            tt(pstart[:], cumb[:], nblk[:], ALU.subtract, [cumb, nblk], [pstart])
            ts(pstart[:], pstart[:], 128.0, None, ALU.mult, None, [pstart], [pstart])
            bsi = k.sb("bsi", [128, NE, 4], I32); bsf = k.sb("bsf", [128, NE, 4], F32); vld = k.sb("vld", [128, NE, 4], F32); jv = k.sb("jv", [128, NE, 4], F32)
            k.op("pool", lambda e: e.iota(bsi[:], pattern=[[0, NE], [128, 4]], base=0, channel_multiplier=1), writes=[bsi])
            cp(k, "dve", bsf[:], bsi[:], [bsi], [bsf])
            tt(bsf[:], bsf[:], pstart[:].unsqueeze(2).to_broadcast([128, NE, 4]), ALU.add, [bsf, pstart], [bsf])
            k.op("pool", lambda e: e.iota(bsi[:], pattern=[[0, NE], [1, 4]], base=0, channel_multiplier=0), reads=[bsi], writes=[bsi])
            cp(k, "dve", jv[:], bsi[:], [bsi], [jv])
            tt(vld[:], nblk[:].unsqueeze(2).to_broadcast([128, NE, 4]), jv[:], ALU.is_le, [nblk, jv], [vld])
            k.op("dve", lambda e: e.scalar_tensor_tensor(out=vld[:], in0=vld[:], scalar=1.0e6, in1=bsf[:], op0=ALU.mult, op1=ALU.add), reads=[vld, bsf], writes=[vld])
            cp(k, "dve", idxout[:], vld[:], [vld], [idxout])
            ts(bsf[:], bsf[:], float(NROW - 1), None, ALU.min, None, [bsf], [bsf])
            cp(k, "dve", idxin[:], bsf[:], [bsf], [idxin])
            for it in range(16):
                hb_ = h2b[it % 2]
                for kk in range(2):
                    ts(t32[:], iote[:], eidk[:, it, kk:kk + 1], None, ALU.is_equal, None, [iote, eidk], [t32])
                    tt(t32[:], t32[:], pstart[:], ALU.mult, [t32, pstart], [t32])
                    red(df[:], t32[:], ALU.add, [t32], [df])
                    tt(df[:], df[:], posk[:, it, kk:kk + 1], ALU.add, [df, posk], [df])
                    cp(k, "dve", dest_i[:, it, kk:kk + 1], df[:], [df, dest_i], [dest_i])
                k.dma("sp", [(hb_[:], H2_d[it * 128:(it + 1) * 128, :])], hb_, reads=[H2_d], writes=[hb_])
                k.dma_fn("pool", [(lambda e, kk=kk, hb_=hb_, it=it: e.indirect_dma_start(out=H_d[:, :], out_offset=bass.IndirectOffsetOnAxis(ap=dest_i[:, it, kk:kk + 1], axis=0),
                                                                                in_=hb_[:, :], in_offset=None, bounds_check=bcreg(e), oob_is_err=False)) for kk in range(2)],
                         hb_, reads=[hb_, dest_i], writes=[H_d])
            if "r" in dbg:
                k.dma("sp", [(outp("dbg_dest", [128, 32], I32), dest_i[:].rearrange("p a b -> p (a b)"))], dest_i, reads=[dest_i])
                k.dma("sp", [(outp("dbg_wts", [128, 32]), wts[:].rearrange("p a b -> p (a b)"))], wts, reads=[wts])
                k.dma("sp", [(outp("dbg_cnt", [128, 32]), cntb[:])], cntb, reads=[cntb])
                k.dma("sp", [(outp("dbg_idxin", [128, NE * 4], I32), idxin[:].rearrange("p a b -> p (a b)"))], idxin, reads=[idxin])
                k.dma("sp", [(outp("dbg_idxout", [128, NE * 4], I32), idxout[:].rearrange("p a b -> p (a b)"))], idxout, reads=[idxout])
        if "x1" in dbg:
            with k.phase():
                o = outp("dbg_x1", [NOWN, D], F32)
                tmp = k.sb("dbgtx", [128, 16, D], F32)
                k.dma("sp", [(tmp[:], X1_d[:].rearrange("(t p) n -> p t n", p=128))], tmp, reads=[X1_d], writes=[tmp])
                k.dma("sp", [(o.rearrange("(t p) n -> p t n", p=128), tmp[:])], tmp, reads=[tmp])

        if upto >= 6 and "skip6" not in dbg:
          with k.phase():
            W1s = [k.sb("W1s%d" % i, [128, 16, DE], BF16) for i in range(2)]
            W3s = [k.sb("W3s%d" % i, [128, 16, DE], BF16) for i in range(2)]
            W2s = [k.sb("W2s%d" % i, [128, 4, D], BF16) for i in range(2)]
            xin4 = [k.sb("xin4_%d" % j, [128, D], BF16) for j in range(4)]
            xT = [k.sb("xT%d" % i, [128, 16, 512], BF16) for i in range(2)]
            slu = [k.sb("slu%d" % i, [128, 512], F32) for i in range(2)]
            gT = [k.sb("gT%d" % i, [128, 4, 512], BF16) for i in range(2)]
            Yt = [[k.sb("Yt%d_%d" % (i, hh), [128, D // 2], F32) for hh in range(2)] for i in range(2)]
            pXT = [k.ps("pXT%d" % i, [128, 8, 128], BF16) for i in range(2)]
            pH1 = [k.ps("pH1_%d" % i, [128, 512], F32) for i in range(2)]
            pH3 = [k.ps("pH3_%d" % i, [128, 512], F32) for i in range(2)]
            pYo = [k.ps("pYo%d" % i, [128, 512], F32) for i in range(2)]
            nev = 0
            nyo = 0
            nyt = 0
            for ex in range(NE):
                ws_ = ex % 2
                xt_ = xT[ws_]
                g_ = gT[ws_]
                k.dma("pool", [(W1s[ws_][:, 0:8, :], w1[ex].rearrange("(kc p) n -> p kc n", p=128)[:, 0:8, :]), (W1s[ws_][:, 8:16, :], w1[ex].rearrange("(kc p) n -> p kc n", p=128)[:, 8:16, :])], W1s[ws_], writes=[W1s[ws_]])
                k.dma("pool", [(W3s[ws_][:, 0:8, :], w3[ex].rearrange("(kc p) n -> p kc n", p=128)[:, 0:8, :]), (W3s[ws_][:, 8:16, :], w3[ex].rearrange("(kc p) n -> p kc n", p=128)[:, 8:16, :])], W3s[ws_], writes=[W3s[ws_]])
                k.dma("pool", [(W2s[ws_][:, 0:2, :], w2[ex].rearrange("(hc p) n -> p hc n", p=128)[:, 0:2, :]), (W2s[ws_][:, 2:4, :], w2[ex].rearrange("(hc p) n -> p hc n", p=128)[:, 2:4, :])], W2s[ws_], writes=[W2s[ws_]])
                for j in range(4):
                    xi_ = xin4[j]
                    k.dma_fn("pool", [lambda e, xi_=xi_, ex=ex, j=j: e.indirect_dma_start(out=xi_[:, :], out_offset=None, in_=H_d[:, :],
                                                                                        in_offset=bass.IndirectOffsetOnAxis(ap=idxin[:, ex, j:j + 1], axis=0))],
                             xi_, reads=[H_d, idxin], writes=[xi_])
                for j in range(4):
                    xi_ = xin4[j]
                    for half in range(2):
                        px = pXT[nev % 2]
                        for c in range(8):
                            kc = half * 8 + c
                            k.op("pe", lambda e, px=px, c=c, kc=kc, xi_=xi_: e.transpose(out=px[:, c, :], in_=xi_[:, kc * 128:(kc + 1) * 128], identity=ident[:]), reads=[xi_, ident], writes=[px], inc=(c == 7))
                        cp(k, "act" if nev % 2 == 0 else "dve", xt_[:, half * 8:(half + 1) * 8, j * 128:(j + 1) * 128], px[:], [px], [xt_])
                        nev += 1
                for hc in range(4):
                    hs_ = hc % 2
                    for (Wt, ph) in ((W1s[ws_], pH1[hs_]), (W3s[ws_], pH3[hs_])):
                        for kc in range(16):
                            k.op("pe", lambda e, ph=ph, hc=hc, kc=kc, Wt=Wt, xt_=xt_: e.matmul(ph[:], lhsT=Wt[:, kc, hc * 128:(hc + 1) * 128], rhs=xt_[:, kc, :], start=(kc == 0), stop=(kc == 15)),
                                 reads=[Wt, xt_], writes=[ph], inc=(kc == 15))
                    k.op("act", lambda e, hs_=hs_: e.activation(out=slu[hs_][:], in_=pH1[hs_][:], func=AF.Silu), reads=[pH1[hs_]], writes=[slu[hs_]])
                    k.op("dve", lambda e, hs_=hs_, hc=hc, g_=g_: e.tensor_tensor(out=g_[:, hc, :], in0=slu[hs_][:], in1=pH3[hs_][:], op=ALU.mult), reads=[slu[hs_], pH3[hs_]], writes=[g_])
                for j in range(4):
                    yt_ = Yt[nyt % 2]
                    nyt += 1
                    for nb in range(4):
                        po_ = pYo[nyo % 2]
                        nyo += 1
                        for hc in range(4):
                            k.op("pe", lambda e, po_=po_, nb=nb, hc=hc, ws_=ws_, g_=g_, j=j: e.matmul(po_[:], lhsT=g_[:, hc, j * 128:(j + 1) * 128], rhs=W2s[ws_][:, hc, nb * 512:(nb + 1) * 512], start=(hc == 0), stop=(hc == 3)),
                                 reads=[g_, W2s[ws_]], writes=[po_], inc=(hc == 3))
                        cp(k, "act" if nb % 2 == 0 else "dve", yt_[nb // 2][:, (nb % 2) * 512:(nb % 2 + 1) * 512], po_[:], [po_], [yt_[nb // 2]])
                        if nb % 2 == 1:
                            hh = nb // 2
                            Yd_ = (Y_dA, Y_dB)[hh]
                            k.dma_fn("pool", [lambda e, yt_=yt_, ex=ex, j=j, Yd_=Yd_, hh=hh: e.indirect_dma_start(out=Yd_[:, :], out_offset=bass.IndirectOffsetOnAxis(ap=idxout[:, ex, j:j + 1], axis=0),
                                                                                            in_=yt_[hh][:, :], in_offset=None, bounds_check=bcreg(e), oob_is_err=False)],
                                     yt_[hh], reads=[yt_[hh], idxout], writes=[Yd_])

        if upto >= 7:
          out_own = outp("y_out", [NOWN, D], F32)
          with k.phase():
            wpg = k.sb("wpg", [128, 16, D], BF16)
            wpg_v = w_ple_gate.rearrange("(kc p) n -> p kc n", p=128)
            k.dma("pool", [(wpg[:, 4 * q:4 * q + 4, :], wpg_v[:, 4 * q:4 * q + 4, :]) for q in range(4)], wpg, writes=[wpg])
            wpp = k.sb("wpp", [128, 2, D], BF16)
            k.dma("pool", [(wpp[:], w_ple_proj.rearrange("(c p) n -> p c n", p=128))], wpp, writes=[wpp])
            gpl = k.sb("gpl", [128, D], F32); gfin = k.sb("gfin", [128, D], F32)
            k.dma("sp", [(gpl[:], g_ple.partition_broadcast(128))], gpl, writes=[gpl])
            k.dma("sp", [(gfin[:], g_final.partition_broadcast(128))], gfin, writes=[gfin])
            x1s = [k.sb("x1s%d" % i, [128, D], F32) for i in range(2)]
            y1s = [k.sb("y1s%d" % i, [128, D], F32) for i in range(2)]
            y2s = [k.sb("y2s%d" % i, [128, D], F32) for i in range(2)]
            x3 = k.sb("x3", [128, D], F32); ot = [k.sb("ot%d" % i, [128, D], F32) for i in range(2)]
            h3b = k.sb("h3b", [128, D], BF16); h3T = k.sb("h3T", [128, 16, 128], BF16)
            pt = [k.sb("pt%d" % i, [128, 256], F32) for i in range(2)]; ptb = k.sb("ptb", [128, 256], BF16); pTT = k.sb("pTT", [128, 2, 128], BF16)
            sgt = k.sb("sgt", [128, 512], F32); tg = k.sb("tg", [128, 512], F32)
            jk7 = k.sb("jk7", [128, D], BF16); ms7 = k.sb("ms7", [128, 1], F32)
            pT7 = [k.ps("pT7%d" % i, [128, 8, 128], BF16) for i in range(2)]
            pGa = [k.ps("pGa%d" % i, [128, 512], F32) for i in range(2)]
            pPp = [k.ps("pPp%d" % i, [128, 512], F32) for i in range(2)]
            def rms(xb, x_ap, gb, out_b, out_ap):
                k.op("act", lambda e: e.activation(out=jk7[:], in_=x_ap, func=AF.Square, accum_out=ms7[:]), reads=[xb], writes=[jk7, ms7])
                k.op("dve", lambda e: e.tensor_scalar(out=ms7[:], in0=ms7[:], scalar1=1.0 / D, scalar2=EPS, op0=ALU.mult, op1=ALU.add), reads=[ms7], writes=[ms7])
                k.op("act", lambda e: e.activation(out=ms7[:], in_=ms7[:], func=AF.Sqrt), reads=[ms7], writes=[ms7])
                k.op("dve", lambda e: e.reciprocal(out=ms7[:], in_=ms7[:]), reads=[ms7], writes=[ms7])
                k.op("dve", lambda e: e.scalar_tensor_tensor(out=out_ap, in0=x_ap, scalar=ms7[:], in1=gb[:], op0=ALU.mult, op1=ALU.mult), reads=[xb, ms7, gb], writes=[out_b])
            for it in range(16):
                s_ = it % 2
                xa, ya, yb, pa, oa = x1s[s_], y1s[s_], y2s[s_], pt[s_], ot[s_]
                k.dma("sp", [(xa[:], X1_d[it * 128:(it + 1) * 128, :])], xa, reads=[X1_d], writes=[xa])
                k.dma("sp", [(pa[:], po[it * 128:(it + 1) * 128, :])], pa, writes=[pa])
                for (yy, kk) in ((ya, 0), (yb, 1)):
                    if "nogather" in dbg:
                        continue
                    k.dma_fn("pool", [lambda e, yy=yy, kk=kk, it=it, Yd_=Yd_, hh=hh: e.indirect_dma_start(out=yy[:, hh * 1024:(hh + 1) * 1024], out_offset=None, in_=Yd_[:, :],
                                                                                        in_offset=bass.IndirectOffsetOnAxis(ap=dest_i[:, it, kk:kk + 1], axis=0))
                                      for hh, Yd_ in enumerate((Y_dA, Y_dB))],
                             yy, reads=[Y_dA, Y_dB, dest_i, yy], writes=[yy])
                k.op("dve", lambda e, xa=xa, ya=ya, it=it: e.scalar_tensor_tensor(out=xa[:], in0=ya[:], scalar=wts[:, it, 0:1], in1=xa[:], op0=ALU.mult, op1=ALU.add), reads=[ya, wts, xa], writes=[xa])
                k.op("dve", lambda e, xa=xa, yb=yb, it=it: e.scalar_tensor_tensor(out=xa[:], in0=yb[:], scalar=wts[:, it, 1:2], in1=xa[:], op0=ALU.mult, op1=ALU.add), reads=[yb, wts, xa], writes=[xa])
                if "st1" in dbg:
                    k.dma("act", [(out_own[it * 128:(it + 1) * 128, :], xa[:])], xa, reads=[xa])
                    continue
                rms(xa, xa[:], gpl, h3b, h3b[:])
                if "st2" in dbg:
                    k.dma("act", [(out_own[it * 128:(it + 1) * 128, :], xa[:])], xa, reads=[xa])
                    continue
                for half in range(2):
                    for c in range(8):
                        kc = half * 8 + c
                        k.op("pe", lambda e, half=half, c=c, kc=kc: e.transpose(out=pT7[half][:, c, :], in_=h3b[:, kc * 128:(kc + 1) * 128], identity=ident[:]), reads=[h3b, ident], writes=[pT7[half]], inc=(c == 7))
                    cp(k, "act" if half == 0 else "dve", h3T[:, half * 8:(half + 1) * 8, :], pT7[half][:], [pT7[half]], [h3T])
                cp(k, "pool", ptb[:], pa[:], [pa], [ptb])
                for c in range(2):
                    k.op("pe", lambda e, c=c: e.transpose(out=pT7[0][:, c, :], in_=ptb[:, c * 128:(c + 1) * 128], identity=ident[:]), reads=[ptb, ident], writes=[pT7[0]], inc=(c == 1))
                cp(k, "act", pTT[:], pT7[0][:, 0:2, :], [pT7[0]], [pTT])
                for nb in range(4):
                    pg_, pq_ = pGa[nb % 2], pPp[nb % 2]
                    cs = slice(nb * 512, (nb + 1) * 512)
                    for kc in range(16):
                        k.op("pe", lambda e, pg_=pg_, kc=kc, cs=cs: e.matmul(pg_[:], lhsT=h3T[:, kc, :], rhs=wpg[:, kc, cs], start=(kc == 0), stop=(kc == 15)), reads=[h3T, wpg], writes=[pg_], inc=(kc == 15))
                    for c in range(2):
                        k.op("pe", lambda e, pq_=pq_, c=c, cs=cs: e.matmul(pq_[:], lhsT=pTT[:, c, :], rhs=wpp[:, c, cs], start=(c == 0), stop=(c == 1)), reads=[pTT, wpp], writes=[pq_], inc=(c == 1))
                    k.op("act", lambda e, pg_=pg_: e.activation(out=sgt[:], in_=pg_[:], func=AF.Sigmoid), reads=[pg_], writes=[sgt])
                    k.op("dve", lambda e, pq_=pq_: e.tensor_tensor(out=tg[:], in0=sgt[:], in1=pq_[:], op=ALU.mult), reads=[sgt, pq_], writes=[tg])
                    k.op("pool", lambda e, cs=cs, xa=xa: e.tensor_tensor(out=x3[:, cs], in0=tg[:], in1=xa[:, cs], op=ALU.add), reads=[tg, xa], writes=[x3])
                rms(x3, x3[:], gfin, oa, oa[:])
                k.dma("act", [(out_own[it * 128:(it + 1) * 128, :], oa[:])], oa, reads=[oa])

        if "z" in dbg:
            for nm, t in (("QT", QT_d), ("KT", KT_d)):
              with k.phase():
                o = outp("dbg_" + nm, [NH, 128, S], BF16)
                tmp = k.sb("dbgt" + nm, [128, NH, S], BF16)
                k.dma("sp", [(tmp[:], t[:].rearrange("h p n -> p h n"))], tmp, reads=[t], writes=[tmp])
                k.dma("sp", [(o.rearrange("h p n -> p h n"), tmp[:])], tmp, reads=[tmp])
            with k.phase():
                o = outp("dbg_V", [S, AW], BF16)
                tmp = k.sb("dbgtV", [128, NT, AW], BF16)
                k.dma("sp", [(tmp[:], V_d[:].rearrange("(t p) n -> p t n", p=128))], tmp, reads=[V_d], writes=[tmp])
                k.dma("sp", [(o.rearrange("(t p) n -> p t n", p=128), tmp[:])], tmp, reads=[tmp])
            with k.phase():
                o = outp("dbg_U", [128, NG * 512], BF16)
                tmp = k.sb("dbgtU", [128, NG * 512], BF16)
                k.dma("sp", [(tmp[:], U_d[:].rearrange("s h g c -> (s h) (g c)"))], tmp, reads=[U_d], writes=[tmp])
                k.dma("sp", [(o, tmp[:])], tmp, reads=[tmp])

        if not O:
            k.dma("sp", [(outp("dummy_out", [128, 4]), ccs[:])], ccs, reads=[ccs])
        k.finish(final)
    return nc, I, O


_NC_CACHE = {}


def _core_inputs(inp, c):
    b, hf = c // 2, c % 2
    perm = own_token_perm(hf)
    m = {}
    xb = np.ascontiguousarray(inp["x"][b], dtype=np.float32)
    m["xg"] = xb
    m["xo"] = np.ascontiguousarray(xb[perm])
    m["po"] = np.ascontiguousarray(np.asarray(inp["p"][0, b], dtype=np.float32)[perm])
    cc = np.zeros((128, 4), np.float32)
    cc[:, 0] = 1 - hf
    cc[:, 1] = hf
    m["cc"] = cc
    m["ohlag"] = _lag_onehot(hf)
    f = lambda a: np.ascontiguousarray(np.asarray(a, dtype=np.float32))
    m["rel_bias"] = f(inp["rel_bias"])
    m["lamv"] = f(np.stack([inp["lam_q1"][0], inp["lam_k1"][0], inp["lam_q2"][0], inp["lam_k2"][0]]))
    for nm in ("g_mix", "w_in", "subln_g", "ssm_lam_re", "ssm_lam_im", "ssm_log_dt", "ssm_b_re", "ssm_b_im", "ssm_c_re", "ssm_c_im",
               "ssm_d", "w_glu", "b_glu", "ssm_norm_g", "w_o", "g_ffn", "w1", "w3", "w2", "g_ple", "w_ple_gate", "w_ple_proj"):
        m[nm] = f(inp[nm][0])
    m["g_final"] = f(inp["g_final"])
    m["wr"] = f(np.concatenate([inp["w_router_g"][0], inp["w_router_e"][0]], axis=1))
    m["br"] = f(np.concatenate([inp["b_router_g"][0], inp["b_router_e"][0]]))
    return m


def kernel(**inputs):
    if "nc" not in _NC_CACHE:
        _NC_CACHE["nc"] = build_nc()
    nc, I, O = _NC_CACHE["nc"]
    in_maps = []
    for c in range(8):
        m = _core_inputs(inputs, c)
        in_maps.append({k_: v for k_, v in m.items() if k_ in I})
    res = run_bass_kernel_spmd(nc, in_maps, core_ids=list(range(8)))
    out = np.zeros((4, S, D), np.float32)
    for c in range(8):
        b, hf = c // 2, c % 2
        out[b][own_token_perm(hf)] = np.asarray(res.results[c]["y_out"], dtype=np.float32)
    return out
```
